# Optimizing a Trainium2 kernel written in Bass

```python
import math
import jax
import jax.numpy as jnp
from jax import lax
import numpy as np

D_MODEL = 2048
BATCH = 4
SEQ = 4096
DEPTH = 1

HEAD_DIM = 128
ROPE_DIM = HEAD_DIM // 4
NOPE_DIM = HEAD_DIM - ROPE_DIM
ROPE_THETA = 500000.0
EPS = 1e-6
NEG = -1e30

N_HEADS_A = 8
WIDTH_A = N_HEADS_A * HEAD_DIM
Q_RANK = 512
KV_RANK = 256
IDX_HEADS = 16
IDX_DIM = 64
IDX_ROPE = IDX_DIM // 4
TOPK_MAX = 256
QUERY_BLOCK = 128

N_HEADS_B = 8
WIDTH_B = N_HEADS_B * HEAD_DIM
DILATED_CONFIGS = ((128, 1), (512, 4), (2048, 16))
BAND_BLOCK = 128

MIX_WIDTH = WIDTH_A + WIDTH_B
IN_SPLITS = (Q_RANK, KV_RANK, ROPE_DIM, IDX_DIM, IDX_HEADS, WIDTH_A,
             WIDTH_B, WIDTH_B, WIDTH_B, WIDTH_B)
IN_WIDTH = sum(IN_SPLITS)

kernel_name = 'hybrid_dsa_dilated_parallel_heads'


def rms_norm(x, g):
    xf = x.astype(jnp.float32)
    y = xf * lax.rsqrt(jnp.mean(xf * xf, axis=-1, keepdims=True) + EPS)
    return (y * g.astype(jnp.float32)).astype(x.dtype)


def partial_rope(x, n_rot):
    L = x.shape[1]
    inv = ROPE_THETA ** (-jnp.arange(0, n_rot, 2, dtype=jnp.float32) / n_rot)
    ang = jnp.arange(L, dtype=jnp.float32)[:, None] * inv[None, :]
    cos = jnp.cos(ang).astype(x.dtype)[None, :, None, :]
    sin = jnp.sin(ang).astype(x.dtype)[None, :, None, :]
    half = n_rot // 2
    x1 = x[..., :half]
    x2 = x[..., half:n_rot]
    return jnp.concatenate([x1 * cos - x2 * sin, x2 * cos + x1 * sin, x[..., n_rot:]], axis=-1)


def dsa_branch(cq, ckv, krope, kidx, widx, g_q, g_kv, w_uq, w_uq_idx, w_uk, w_uv):
    Bsz, L, _ = cq.shape
    k_sel = min(TOPK_MAX, L // 4)
    nb = L // QUERY_BLOCK
    scale = HEAD_DIM ** -0.5
    cq = rms_norm(cq, g_q)
    q = partial_rope((cq @ w_uq).reshape(Bsz, L, N_HEADS_A, HEAD_DIM), ROPE_DIM)
    q_rope, q_nope = q[..., :ROPE_DIM], q[..., ROPE_DIM:]
    q_lat = jnp.einsum('blhn,hnr->blhr', q_nope, w_uk)
    q_idx = partial_rope((cq @ w_uq_idx).reshape(Bsz, L, IDX_HEADS, IDX_DIM), IDX_ROPE)
    w_idx = widx * (IDX_HEADS ** -0.5 * IDX_DIM ** -0.5)
    c_kv = rms_norm(ckv, g_kv)
    k_rope = partial_rope(krope[:, :, None, :], ROPE_DIM)[:, :, 0]
    k_idx = partial_rope(kidx[:, :, None, :], IDX_ROPE)[:, :, 0]
    key_pos = jnp.arange(L)
    gather = jax.vmap(lambda a, i: a[i])

    def attend_block(blk):
        ql, qr, qi, wi, qpos = blk
        logits = jnp.einsum('bqhd,bsd->bqhs', qi, k_idx)
        isc = jnp.einsum('bqh,bqhs->bqs', wi, jax.nn.relu(logits)).astype(jnp.float32)
        causal = key_pos[None, :] <= qpos[:, None]
        isc = jnp.where(causal[None], isc, NEG)
        _, idx = lax.top_k(isc, k_sel)
        valid = idx <= qpos[None, :, None]
        ckv_sel = gather(c_kv, idx)
        kr_sel = gather(k_rope, idx)
        s = (jnp.einsum('bqhr,bqkr->bqhk', ql, ckv_sel)
             + jnp.einsum('bqhe,bqke->bqhk', qr, kr_sel)).astype(jnp.float32) * scale
        s = jnp.where(valid[:, :, None, :], s, NEG)
        p = jax.nn.softmax(s, axis=-1).astype(ckv_sel.dtype)
        return jnp.einsum('bqhk,bqkr->bqhr', p, ckv_sel)

    def to_blocks(t):
        return jnp.moveaxis(t.reshape(Bsz, nb, QUERY_BLOCK, *t.shape[2:]), 1, 0)

    qpos = key_pos.reshape(nb, QUERY_BLOCK)
    o_lat = lax.map(attend_block, (to_blocks(q_lat), to_blocks(q_rope), to_blocks(q_idx),
                                   to_blocks(w_idx), qpos))
    o_lat = jnp.moveaxis(o_lat, 0, 1).reshape(Bsz, L, N_HEADS_A, KV_RANK)
    return jnp.einsum('blhr,hrv->blhv', o_lat, w_uv).reshape(Bsz, L, WIDTH_A)


def dilated_window_attention(q, k, v, dil, n_back):
    Bsz, L, H, Dh = q.shape
    M = L // dil
    N = Bsz * dil
    P = BAND_BLOCK
    nb = -(-M // P)
    Mp = nb * P
    scale = Dh ** -0.5

    def to_res(t):
        return jnp.swapaxes(t.reshape(Bsz, M, dil, H, Dh), 1, 2).reshape(N, M, H, Dh)

    def key_blocks(t):
        tp = jnp.pad(t, ((0, 0), (P, Mp - M), (0, 0), (0, 0))).reshape(N, nb + 1, P, H, Dh)
        return jnp.concatenate([tp[:, :-1], tp[:, 1:]], axis=2)

    qb = jnp.pad(to_res(q), ((0, 0), (0, Mp - M), (0, 0), (0, 0))).reshape(N, nb, P, H, Dh)
    kb = key_blocks(to_res(k))
    vb = key_blocks(to_res(v))
    qpos = jnp.arange(Mp).reshape(nb, P)
    kpos = jnp.arange(nb)[:, None] * P - P + jnp.arange(2 * P)[None, :]
    rel = qpos[:, :, None] - kpos[:, None, :]
    allowed = (rel >= 0) & (rel <= n_back) & (kpos[:, None, :] >= 0)
    s = jnp.einsum('nbqhd,nbkhd->nbhqk', qb, kb).astype(jnp.float32) * scale
    s = jnp.where(allowed[None, :, None], s, NEG)
    lse = jax.nn.logsumexp(s, axis=-1)
    p = jnp.exp(s - lse[..., None]).astype(v.dtype)
    o = jnp.einsum('nbhqk,nbkhd->nbqhd', p, vb).reshape(N, Mp, H, Dh)[:, :M]
    lse = jnp.swapaxes(lse, 2, 3).reshape(N, Mp, H)[:, :M]
    o = jnp.swapaxes(o.reshape(Bsz, dil, M, H, Dh), 1, 2).reshape(Bsz, L, H, Dh)
    lse = jnp.swapaxes(lse.reshape(Bsz, dil, M, H), 1, 2).reshape(Bsz, L, H)
    return o, lse


def dilated_branch(qb, kb, vb):
    Bsz, L, _ = qb.shape
    q = partial_rope(qb.reshape(Bsz, L, N_HEADS_B, HEAD_DIM), ROPE_DIM)
    k = partial_rope(kb.reshape(Bsz, L, N_HEADS_B, HEAD_DIM), ROPE_DIM)
    v = vb.reshape(Bsz, L, N_HEADS_B, HEAD_DIM)
    outs, lses = [], []
    for window, dil in DILATED_CONFIGS:
        o, lse = dilated_window_attention(q, k, v, dil, window // dil)
        outs.append(o)
        lses.append(lse)
    alpha = jax.nn.softmax(jnp.stack(lses), axis=0)
    o = jnp.sum(alpha[..., None] * jnp.stack(outs).astype(jnp.float32), axis=0)
    return o.astype(q.dtype).reshape(Bsz, L, WIDTH_B)


def hybrid_layer(x, c, w_ada, b_ada, g_pre, g_post, w_in, g_q, g_kv, w_uq, w_uq_idx,
                 w_uk, w_uv, w_out):
    mod = jax.nn.silu(c) @ w_ada + b_ada
    shift, scale, gate = jnp.split(mod, 3, axis=-1)
    h = rms_norm(x, g_pre) * (1 + scale[:, None, :]) + shift[:, None, :]
    proj = h @ w_in
    split_points = [int(v) for v in np.cumsum(IN_SPLITS)[:-1]]
    (cq, ckv, krope, kidx, widx, gate_a, qb, kb, vb, gate_b) = jnp.split(proj, split_points, axis=-1)
    o_a = dsa_branch(cq, ckv, krope, kidx, widx, g_q, g_kv, w_uq, w_uq_idx, w_uk, w_uv) * jax.nn.silu(gate_a)
    o_b = dilated_branch(qb, kb, vb) * jax.nn.silu(gate_b)
    y = jnp.concatenate([o_a, o_b], axis=-1) @ w_out
    return x + gate[:, None, :] * rms_norm(y, g_post)


def setup_inputs(seed: int = 0) -> dict:
    key = jax.random.key(seed)
    ks = jax.random.split(key, 14)

    def nrm(k, shape, s):
        return jax.random.normal(k, shape, jnp.float32) * s

    return {
        'x': nrm(ks[0], (BATCH, SEQ, D_MODEL), 1.0),
        'c': nrm(ks[1], (BATCH, D_MODEL), 1.0),
        'w_ada': nrm(ks[2], (DEPTH, D_MODEL, 3 * D_MODEL), D_MODEL ** -0.5),
        'b_ada': nrm(ks[3], (DEPTH, 3 * D_MODEL), 0.01),
        'g_pre': 1.0 + nrm(ks[4], (DEPTH, D_MODEL), 0.01),
        'g_post': 1.0 + nrm(ks[5], (DEPTH, D_MODEL), 0.01),
        'w_in': nrm(ks[6], (DEPTH, D_MODEL, IN_WIDTH), D_MODEL ** -0.5),
        'g_q': 1.0 + nrm(ks[7], (DEPTH, Q_RANK), 0.01),
        'g_kv': 1.0 + nrm(ks[8], (DEPTH, KV_RANK), 0.01),
        'w_uq': nrm(ks[9], (DEPTH, Q_RANK, WIDTH_A), Q_RANK ** -0.5),
        'w_uq_idx': nrm(ks[10], (DEPTH, Q_RANK, IDX_HEADS * IDX_DIM), Q_RANK ** -0.5),
        'w_uk': nrm(ks[11], (DEPTH, N_HEADS_A, NOPE_DIM, KV_RANK), KV_RANK ** -0.5),
        'w_uv': nrm(ks[12], (DEPTH, N_HEADS_A, KV_RANK, HEAD_DIM), KV_RANK ** -0.5),
        'w_out': nrm(ks[13], (DEPTH, MIX_WIDTH, D_MODEL), MIX_WIDTH ** -0.5),
    }


def reference(x, c, w_ada, b_ada, g_pre, g_post, w_in, g_q, g_kv, w_uq, w_uq_idx,
              w_uk, w_uv, w_out):
    for layer in range(DEPTH):
        x = hybrid_layer(x, c, w_ada[layer], b_ada[layer], g_pre[layer], g_post[layer],
                         w_in[layer], g_q[layer], g_kv[layer], w_uq[layer], w_uq_idx[layer],
                         w_uk[layer], w_uv[layer], w_out[layer])
    return x
```

```python
import contextlib
import numpy as np
import concourse.bass as bass
import concourse.mybir as mybir
from concourse.bass_utils import run_bass_kernel_spmd

F32 = mybir.dt.float32
BF16 = mybir.dt.bfloat16
AF = mybir.ActivationFunctionType
ALU = mybir.AluOpType
AX = mybir.AxisListType

T = 4096
D = 2048
KC = 16
NCH = 8
NTT = 32
EPS = 1e-6
THETA = 500000.0
NIT = 20
OT0 = 16
OC0 = 4
NEG = -1.0e30
SCALE = 128.0 ** -0.5


class Res:
    __slots__ = ("name", "writers", "readers")

    def __init__(self, name):
        self.name = name
        self.writers = {}
        self.readers = []


class Op:
    __slots__ = ("eng", "fn", "deps", "users", "sem", "val", "is_dma", "slot")

    def __init__(self, eng, fn):
        self.eng = eng
        self.fn = fn
        self.deps = []
        self.users = 0
        self.sem = None
        self.val = 0
        self.is_dma = False
        self.slot = None


class Sched:
    ENGS = ("pe", "act", "dve", "pool", "sp")

    def __init__(self):
        self.ops = {e: [] for e in self.ENGS}
        self.slots = {}
        self.last = {e: None for e in self.ENGS}
        self.pending_dma = []
        self.slotinfo = {}

    def add(self, eng, fn, reads=(), writes=(), dma_slot=None):
        op = Op(eng, fn)
        raw = []
        oth = []
        for r in reads:
            raw.extend(r.writers.values())
        for w in writes:
            if eng != "pe":
                raw.extend(w.writers.values())
            else:
                oth.extend(w.writers.values())
            oth.extend(w.readers)
        seen = set()
        for d in raw:
            if id(d) in seen or d is op:
                continue
            if d.eng == eng and not d.is_dma and eng == "pe":
                continue
            seen.add(id(d))
            op.deps.append(d)
        for d in oth:
            if id(d) in seen or d is op:
                continue
            if d.eng == eng and not d.is_dma and eng != "pool" and dma_slot is None:
                continue
            seen.add(id(d))
            op.deps.append(d)
        ups = []
        seen2 = set()
        for d in op.deps:
            if d.is_dma:
                info = self.slotinfo[d.slot]
                d = info["last"]
                info["waited"] = d
            if id(d) not in seen2:
                seen2.add(id(d))
                ups.append(d)
        op.deps = ups
        if dma_slot is not None:
            op.is_dma = True
            op.slot = dma_slot
            info = self.slotinfo.setdefault(dma_slot, {"last": None, "waited": None})
            w_ = info["waited"]
            if w_ is not None and id(w_) not in seen2:
                op.deps.append(w_)
            info["last"] = op
            self.pending_dma.append(op)
        for d in op.deps:
            d.users += 1
        wkey = dma_slot if dma_slot is not None else eng
        for w in writes:
            w.writers[wkey] = op
            w.readers = []
        for r in reads:
            r.readers.append(op)
        self.ops[eng].append(op)
        self.last[eng] = op
        return op

    def barrier(self):
        lasts = [self.last[e] for e in self.ENGS if self.last[e] is not None]
        dmas = []
        for d in self.pending_dma:
            info = self.slotinfo[d.slot]
            d2 = info["last"]
            info["waited"] = d2
            if all(d2 is not x for x in dmas):
                dmas.append(d2)
        self.pending_dma = []
        for e in self.ENGS:
            op = Op(e, lambda eng: eng.nop())
            for d in lasts + dmas:
                if d.eng == e and not d.is_dma and e != "pool":
                    continue
                op.deps.append(d)
                d.users += 1
            self.ops[e].append(op)
            self.last[e] = op

    def emit(self, nc, final_waits=()):
        with contextlib.ExitStack() as st:
            esem = {e: st.enter_context(nc.semaphore("s_" + e)) for e in self.ENGS}
            for e in self.ENGS:
                for op in self.ops[e]:
                    if op.is_dma and op.slot not in self.slots:
                        self.slots[op.slot] = [st.enter_context(nc.semaphore("d_%d" % len(self.slots))), 0]
            for e in self.ENGS:
                cnt = 0
                for op in self.ops[e]:
                    if op.is_dma:
                        s = self.slots[op.slot]
                        s[1] += 16
                        op.sem = s[0]
                        op.val = s[1]
                    elif op.users > 0:
                        cnt += 1
                        op.sem = esem[e]
                        op.val = cnt
            block = st.enter_context(nc.Block())

            def run(e, engine):
                seen = {}
                for op in self.ops[e]:
                    for d in op.deps:
                        k = id(d.sem)
                        if seen.get(k, 0) >= d.val:
                            continue
                        engine.wait_ge(d.sem, d.val)
                        seen[k] = d.val
                    inst = op.fn(engine)
                    if op.is_dma:
                        inst.then_inc(op.sem, 16)
                    elif op.users > 0:
                        inst.then_inc(op.sem, 1)
                if e == "sp":
                    for d in final_waits:
                        k = id(d.sem)
                        if seen.get(k, 0) >= d.val:
                            continue
                        engine.wait_ge(d.sem, d.val)
                        seen[k] = d.val

            @block.tensor
            def _(eng):
                run("pe", eng)

            @block.scalar
            def _(eng):
                run("act", eng)

            @block.vector
            def _(eng):
                run("dve", eng)

            @block.gpsimd
            def _(eng):
                run("pool", eng)

            @block.sync
            def _(eng):
                run("sp", eng)


def _rope_tables(g):
    pos = np.maximum(np.arange(T, dtype=np.float32) + 2048.0 * (g - 1), 0.0).astype(np.float32)
    inv32 = (THETA ** (-np.arange(0, 32, 2, dtype=np.float32) / 32)).astype(np.float32)
    inv16 = (THETA ** (-np.arange(0, 16, 2, dtype=np.float32) / 16)).astype(np.float32)
    a32 = (pos[None, :] * inv32[:, None]).astype(np.float32)
    a16 = (pos[None, :] * inv16[:, None]).astype(np.float32)
    tabs = np.zeros((3, 2, 128, T), np.float32)
    tabs[:, 0] = 1.0
    tabs[0, 0, 0:16] = np.cos(a32); tabs[0, 0, 16:32] = np.cos(a32)
    tabs[0, 1, 0:16] = np.sin(a32); tabs[0, 1, 16:32] = np.sin(a32)
    tabs[1, 0, 0:32] = tabs[0, 0, 0:32]; tabs[1, 1, 0:32] = tabs[0, 1, 0:32]
    tabs[1, 0, 32:40] = np.cos(a16); tabs[1, 0, 40:48] = np.cos(a16)
    tabs[1, 1, 32:40] = np.sin(a16); tabs[1, 1, 40:48] = np.sin(a16)
    for b in (0, 64):
        tabs[2, 0, b:b + 8] = np.cos(a16); tabs[2, 0, b + 8:b + 16] = np.cos(a16)
        tabs[2, 1, b:b + 8] = np.sin(a16); tabs[2, 1, b + 8:b + 16] = np.sin(a16)
    rm = np.zeros((3, 128, 128), np.float32)

    def blk(R, base, half):
        for j in range(half):
            R[base + j + half, base + j] = -1.0
            R[base + j, base + j + half] = 1.0
    blk(rm[0], 0, 16)
    blk(rm[1], 0, 16); blk(rm[1], 32, 8)
    blk(rm[2], 0, 8); blk(rm[2], 64, 8)
    return tabs, rm


def _masks(g):
    k = np.arange(128)[:, None]
    q = np.arange(128)[None, :]
    prev = (k >= q).astype(np.float32)
    cur = (k <= q).astype(np.float32)
    pp = prev * float(g)
    band = np.stack([np.concatenate([prev, cur, prev, cur], axis=1),
                     np.concatenate([pp, cur, prev, cur], axis=1),
                     np.concatenate([pp, cur, pp, cur], axis=1)], axis=0)
    cm = np.where(np.arange(128)[None, :] <= np.arange(128)[:, None], 0.0, NEG).astype(np.float32)
    return band, cm


def build(debug=False):
    nc = bass.Bass("TRN2", target_bir_lowering=False)
    S = Sched()
    okind = "ExternalOutput" if debug else "Internal"

    def din(name, shape, dt=F32):
        return nc.dram_tensor(name, shape, dt, kind="ExternalInput").ap()

    def dscr(name, shape, dt):
        return nc.dram_tensor(name, shape, dt, kind=okind).ap()

    x = din("x", [T, D])
    scol = din("scol", [128, 16])
    w_ada = din("w_ada", [D, 3 * D])
    bada_col = din("bada_col", [128, 32])
    bada_g = din("bada_g", [1, D])
    gpre_col = din("gpre_col", [128, 16])
    gpost = din("gpost", [1, D])
    gq_col = din("gq_col", [128, 4])
    gkv_col = din("gkv_col", [128, 2])
    w_in = din("w_in", [D, 6000])
    w_uq = din("w_uq", [512, 1024])
    w_uqi = din("w_uqi", [512, 1024])
    w_uk = din("w_uk", [8, 96, 256])
    w_uv = din("w_uv", [8, 256, 128])
    w_out = din("w_out", [D, D])
    tabs = din("tabs", [3, 2, 128, T])
    rmats = din("rmats", [3, 128, 128])
    ident_d = din("ident", [128, 128])
    band_d = din("band", [3, 128, 512])
    padb_d = din("padb", [128, 512])
    cm_d = din("cm", [128, 128])
    out = nc.dram_tensor("out", [T // 2, D], F32, kind="ExternalOutput").ap()

    hT_scr = dscr("hT_scr", [NCH, 128, KC * 512], BF16)
    cqg_scr = dscr("cqg_scr", [128, 4, T], BF16)
    rstdq_scr = dscr("rstdq_scr", [1, T], F32)
    ckvT_scr = dscr("ckvT_scr", [128, 2, T], BF16)
    KhT_scr = dscr("KhT_scr", [128, 8, T], BF16)
    VH_scr = dscr("VH_scr", [8, 128, NTT, 128], BF16)
    kri_scr = dscr("kri_scr", [96, T], BF16)
    wiT_scr = dscr("wiT_scr", [16, T], F32)
    gaT_scr = dscr("gaT_scr", [128, 8, T], BF16)
    maskT_scr = dscr("maskT_scr", [NCH, 128, NTT, 512], BF16)
    oT_scr = dscr("oT_scr", [128, 16, T], BF16)

    R = {}

    def res(name):
        if name not in R:
            R[name] = Res(name)
        return R[name]

    with contextlib.ExitStack() as st:
        ps = st.enter_context(nc.psum_tensor("ps", [128, 8, 512], F32))
        psr = [res("psb%d" % i) for i in range(8)]
        gb = [0]

        def gbank(n=5):
            b = gb[0] % n
            gb[0] += 1
            return b

        def psb(b):
            return ps[:, b, :].bitcast(BF16)

        def sbp(name, shape, dt):
            return st.enter_context(nc.sbuf_tensor("sb_" + name, shape, dt))

        ident = sbp("identb", [128, 128], BF16)
        identf = sbp("identf", [128, 128], F32)
        ones = sbp("onesb", [128, 128], BF16)
        rm = sbp("rm", [128, 3, 128], BF16)
        band = sbp("bandb", [128, 3, 512], BF16)
        padb = sbp("padb", [128, 512], F32)
        cm = sbp("cm", [128, 128], F32)
        A1 = sbp("A1", [128, 16], F32)
        B1 = sbp("B1", [128, 16], F32)
        Gbc = sbp("Gbc", [128, D], F32)
        gq = sbp("gq", [128, 4], F32)
        gkv = sbp("gkv", [128, 2], F32)
        arena = sbp("arena", [128, 88576], BF16)

        class Arena:
            def __init__(self):
                self.off = 0

            def reset(self):
                self.off = 0

            def alloc(self, shape, dt):
                n = int(np.prod(shape[1:]))
                nb = n * (4 if dt == F32 else 2)
                nb = (nb + 63) // 64 * 64
                o = self.off
                self.off += nb // 2
                assert self.off <= 88576, self.off
                v = arena[0:shape[0], o:o + nb // 2]
                if dt == F32:
                    v = v.bitcast(F32)[:, 0:n]
                else:
                    v = v[:, 0:n]
                if len(shape) == 3:
                    v = v.rearrange("p (a b) -> p a b", a=shape[1])
                elif len(shape) == 4:
                    v = v.rearrange("p (a b c) -> p a b c", a=shape[1], b=shape[2])
                return v

        AR = Arena()

        def dma(eng, o, i, reads, writes, slot, slow=False):
            if slow:
                return S.add(eng, lambda e: e.dma_start(out=o, in_=i, allow_slow_non_contiguous=True), reads, writes, dma_slot=slot)
            return S.add(eng, lambda e: e.dma_start(out=o, in_=i), reads, writes, dma_slot=slot)

        def mm(o, l, r, start, stop, reads, writes):
            return S.add("pe", lambda e: e.matmul(o, lhsT=l, rhs=r, start=start, stop=stop), reads, writes)

        def tr(o, i, reads, writes, idt=None):
            idt = ident[:] if idt is None else idt
            return S.add("pe", lambda e: e.transpose(o, i, idt), reads, writes)

        def act(o, i, func, reads, writes, scale=1.0, bias=0.0, accum=None):
            if accum is None:
                return S.add("act", lambda e: e.activation(out=o, in_=i, func=func, scale=scale, bias=bias), reads, writes)
            return S.add("act", lambda e: e.activation(out=o, in_=i, func=func, scale=scale, bias=bias, accum_out=accum), reads, writes)

        def tt(eng, o, a, b, op, reads, writes):
            return S.add(eng, lambda e: e.tensor_tensor(out=o, in0=a, in1=b, op=op), reads, writes)

        def ts(eng, o, a, s1, s2, op0, op1, reads, writes, accum=None):
            if accum is None:
                if s2 is None:
                    return S.add(eng, lambda e: e.tensor_scalar(out=o, in0=a, scalar1=s1, scalar2=None, op0=op0), reads, writes)
                return S.add(eng, lambda e: e.tensor_scalar(out=o, in0=a, scalar1=s1, scalar2=s2, op0=op0, op1=op1), reads, writes)
            return S.add(eng, lambda e: e.tensor_scalar(out=o, in0=a, scalar1=s1, scalar2=s2, op0=op0, op1=op1, accum_out=accum), reads, writes)

        def stt(o, a, s, b, op0, op1, reads, writes):
            return S.add("dve", lambda e: e.scalar_tensor_tensor(out=o, in0=a, scalar=s, in1=b, op0=op0, op1=op1), reads, writes)

        def recip(o, i, reads, writes):
            return S.add("dve", lambda e: e.reciprocal(out=o, in_=i), reads, writes)

        def cp(eng, o, i, reads, writes):
            if eng == "act":
                return S.add("act", lambda e: e.copy(out=o, in_=i), reads, writes)
            return S.add(eng, lambda e: e.tensor_copy(out=o, in_=i), reads, writes)

        def memset(eng, o, v, writes):
            return S.add(eng, lambda e: e.memset(o, v), (), writes)

        rc = res("consts")
        dma("pool", ident[:], ident_d, [], [rc], "cpool")
        dma("sp", identf[:], ident_d, [], [rc], "csp")
        dma("pool", rm[:], rmats.rearrange("t p m -> p t m"), [], [rc], "cpool")
        dma("pool", band[:], band_d.rearrange("t p n -> p t n"), [], [rc], "cpool")
        dma("sp", padb[:], padb_d, [], [rc], "csp")
        dma("sp", cm[:], cm_d, [], [rc], "csp")
        dma("sp", gq[:], gq_col, [], [rc], "csp")
        dma("sp", gkv[:], gkv_col, [], [rc], "csp")
        memset("dve", ones[:], 1.0, [rc])

        AR.reset()
        sc = AR.alloc([128, 16], F32)
        screp = AR.alloc([128, 16, 128], F32)
        bcol = AR.alloc([128, 32], F32)
        gpc = AR.alloc([128, 16], F32)
        modc = AR.alloc([128, 32], F32)
        bg = AR.alloc([128, D], F32)
        gp = AR.alloc([128, D], F32)
        wblk = [AR.alloc([128, 16, 512], F32) for _ in range(2)]
        onesf = AR.alloc([128, 128], F32)
        r_sc, r_mod, r_bg = res("sc"), res("modc"), res("bg")
        r_wblk = [res("wblk0"), res("wblk1")]
        dma("sp", sc, scol, [], [r_sc], "p0")
        dma("sp", bcol, bada_col, [], [r_sc], "p0")
        dma("sp", gpc, gpre_col, [], [r_sc], "p0")
        dma("sp", bg, bada_g.partition_broadcast(128), [], [r_bg], "p0")
        dma("sp", gp, gpost.partition_broadcast(128), [], [r_bg], "p0")
        act(sc, sc, AF.Silu, [r_sc], [r_sc])
        memset("pool", onesf, 1.0, [res("onesf")])
        for kc in range(16):
            ts("dve", screp[:, kc, :], onesf, sc[:, kc:kc + 1], None, ALU.mult, None, [r_sc, res("onesf")], [res("screp")])
        mb = 7
        for blk in range(12):
            wb = wblk[blk % 2]
            rw = r_wblk[blk % 2]
            dma("sp" if blk % 2 == 0 else "act", wb, w_ada[:, blk * 512:(blk + 1) * 512].rearrange("(kc p) n -> p kc n", p=128), [], [rw], "wblk%d" % (blk % 2))
            if blk < 8:
                for jj in range(4):
                    j = blk * 4 + jj
                    for kc in range(16):
                        mm(ps[:, mb, j:j + 1], wb[:, kc, jj * 128:(jj + 1) * 128], sc[:, kc:kc + 1], kc == 0, kc == 15, [rw, r_sc], [psr[mb]])
            else:
                b = gbank()
                for kc in range(16):
                    mm(ps[:, b, :], screp[:, kc, :], wb[:, kc, :], kc == 0, kc == 15, [rw, res("screp")], [psr[b]])
                c0 = (blk - 8) * 512
                tt("dve", Gbc[:, c0:c0 + 512], ps[:, b, :], bg[:, c0:c0 + 512], ALU.add, [psr[b], r_bg], [res("Gbc")])
                tt("pool", Gbc[:, c0:c0 + 512], Gbc[:, c0:c0 + 512], gp[:, c0:c0 + 512], ALU.mult, [res("Gbc"), r_bg], [res("Gbc")])
        tt("dve", modc, ps[:, mb, 0:32], bcol, ALU.add, [psr[mb], r_sc], [r_mod])
        cp("dve", B1[:], modc[:, 0:16], [r_mod], [res("AB")])
        ts("dve", modc[:, 16:32], modc[:, 16:32], 1.0, None, ALU.add, None, [r_mod], [r_mod])
        tt("dve", A1[:], modc[:, 16:32], gpc, ALU.mult, [r_mod, r_sc], [res("AB")])
        S.barrier()

        AR.reset()
        Wa = AR.alloc([128, 16, 1904], BF16)
        xt = [AR.alloc([128, D], F32) for _ in range(2)]
        xn = AR.alloc([128, D], BF16)
        hTc = [AR.alloc([128, 16, 512], BF16) for _ in range(2)]
        st1 = AR.alloc([128, 8], F32)
        sq = AR.alloc([128, 512], BF16)
        rstdb = AR.alloc([128, 512], F32)
        cqg = AR.alloc([128, 4, 512], BF16)
        ckvn = AR.alloc([128, 2, 512], BF16)
        WukT = AR.alloc([128, 2, 8, 128], BF16)
        Wukn = AR.alloc([128, 8, 256], BF16)
        Wuv2 = AR.alloc([128, 2, 8, 128], BF16)
        Esel = AR.alloc([128, 128], BF16)
        KhTc = AR.alloc([128, 8, 512], BF16)
        VHc = AR.alloc([128, 4, 1024], BF16)
        xq = AR.alloc([128, 512], F32)
        xb = AR.alloc([128, 512], BF16)
        t1 = AR.alloc([128, 512], F32)
        t2 = AR.alloc([128, 512], F32)
        krio = AR.alloc([128, 512], BF16)
        gao = [AR.alloc([128, 512], BF16) for _ in range(2)]
        cst = [AR.alloc([128, 2, 512], F32) for _ in range(2)]
        r_Wa = res("Wa")
        r_xt = [res("xt0"), res("xt1")]
        r_hTc = [res("hTc0"), res("hTc1")]
        for i in range(4):
            dma("pool", Wa[:, :, i * 476:(i + 1) * 476], w_in[:, i * 476:(i + 1) * 476].rearrange("(kc p) n -> p kc n", p=128), [], [r_Wa], "Wa")
        CQ0, CKV0, KRI0, GA0 = 0, 512, 768, 880
        r_W2 = res("W2")
        dma("pool", Wukn[0:96, :, :], w_uk.rearrange("h n r -> n h r"), [], [res("Wukn")], "W2")
        for j in range(2):
            dma("pool", Wuv2[:, j], w_uv[:, j * 128:(j + 1) * 128, :].rearrange("h p v -> p h v"), [], [r_W2], "W2")
        memset("pool", WukT, 0.0, [r_W2])
        memset("pool", Esel, 0.0, [r_W2])
        cp("pool", Esel[0:32, 0:32], ident[0:32, 0:32], [rc], [r_W2])
        for j in range(2):
            b = gbank()
            pb = psb(b)
            for h in range(8):
                tr(pb[:, h * 96:(h + 1) * 96], Wukn[0:96, h, j * 128:(j + 1) * 128], [res("Wukn"), rc], [psr[b]], idt=ident[0:96, 0:96])
            cp("dve", WukT[:, j, :, 32:128], pb[:, 0:768].rearrange("p (h n) -> p h n", h=8), [psr[b]], [r_W2])

        def rope(src_f32, src_res, typ, cos, sin, cs_res, out_bf, out_res, nrows=128):
            n = src_f32.shape[-1]
            cp("act", xb[:, 0:n], src_f32, [src_res], [res("xb")])
            b = gbank()
            mm(ps[:, b, 0:n], rm[:, typ, :], xb[:, 0:n], True, True, [res("xb"), rc], [psr[b]])
            tt("pool", t1[:, 0:n], src_f32, cos, ALU.mult, [src_res, cs_res], [res("t1")])
            tt("dve", t2[:, 0:n], ps[:, b, 0:n], sin, ALU.mult, [psr[b], cs_res], [res("t2")])
            tt("pool", out_bf, t1[0:nrows, 0:n], t2[0:nrows, 0:n], ALU.add, [res("t1"), res("t2")], [out_res])

        def _chunk_vars(c):
            return hTc[c % 2], r_hTc[c % 2], c * 512, cst[c % 2], res("cst%d" % (c % 2))

        def xproc_gen(c):
            hc, rh, t0, csb, r_cs = _chunk_vars(c)
            dma("sp", csb, tabs[1, :, :, t0:t0 + 512].rearrange("s p n -> p s n"), [], [r_cs], "cst%d" % (c % 2))
            for q4 in range(4):
                ti = c * 4 + q4
                xx = xt[ti % 2]
                rx = r_xt[ti % 2]
                dma("sp", xx, x[ti * 128:(ti + 1) * 128, :], [], [rx], "xt%d" % (ti % 2))
                act(xn, xx, AF.Square, [rx], [res("xn"), res("st1")], accum=st1[:, 0:1])
                act(st1[:, 1:2], st1[:, 0:1], AF.Sqrt, [res("st1")], [res("st1b")], scale=1.0 / D, bias=EPS)
                recip(st1[:, 2:3], st1[:, 1:2], [res("st1b")], [res("st1c")])
                ts("dve", xn, xx, st1[:, 2:3], None, ALU.mult, None, [rx, res("st1c")], [res("xn")])
                for half in range(2):
                    b = gbank()
                    pb = psb(b)
                    for k8 in range(8):
                        kc = half * 8 + k8
                        tr(pb[:, k8 * 128:(k8 + 1) * 128], xn[:, kc * 128:(kc + 1) * 128], [res("xn"), rc], [psr[b]])
                    for k8 in range(8):
                        kc = half * 8 + k8
                        eng = "dve" if k8 % 2 == 0 else "pool"
                        if eng == "pool":
                            act(hc[:, kc, q4 * 128:(q4 + 1) * 128], pb[:, k8 * 128:(k8 + 1) * 128], AF.Identity, [psr[b], res("AB")], [rh], scale=A1[:, kc:kc + 1], bias=B1[:, kc:kc + 1])
                        else:
                            ts("dve", hc[:, kc, q4 * 128:(q4 + 1) * 128], pb[:, k8 * 128:(k8 + 1) * 128], A1[:, kc:kc + 1], B1[:, kc:kc + 1], ALU.mult, ALU.add, [psr[b], res("AB")], [rh])
                yield
            dma("act", hT_scr[c], hc.rearrange("p a b -> p (a b)"), [rh], [res("hT_scr")], "hTs%d" % (c % 2))

            yield

        def proj_gen(c):
            hc, rh, t0, csb, r_cs = _chunk_vars(c)
            def proj(c0, M, b):
                for kc in range(16):
                    mm(ps[0:M, b, :], Wa[:, kc, c0:c0 + M], hc[:, kc, :], kc == 0, kc == 15, [r_Wa, rh], [psr[b]])

            sb_ = 5
            for j in range(4 if c >= OC0 else 0):
                b = gbank()
                proj(CQ0 + j * 128, 128, b)
                act(sq, ps[:, b, :], AF.Square, [psr[b]], [res("sq")])
                mm(ps[:, sb_, :], ones[:], sq, j == 0, j == 3, [res("sq"), rc], [psr[sb_]])
                act(cqg[:, j, :], ps[:, b, :], AF.Identity, [psr[b], rc], [res("cqg")], scale=gq[:, j:j + 1])
                yield
            if c >= OC0:
                act(rstdb, ps[:, sb_, :], AF.Sqrt, [psr[sb_]], [res("rstdb")], scale=1.0 / 512, bias=EPS)
                recip(rstdb, rstdb, [res("rstdb")], [res("rstdb")])
                dma("pool", rstdq_scr[0:1, t0:t0 + 512], rstdb[0:1, :], [res("rstdb")], [res("rstdq_scr")], "rq")
                dma("act", cqg_scr[:, :, t0:t0 + 512], cqg, [res("cqg")], [res("cqg_scr")], "cqgs")
            bk = [6, 7]
            for j in range(2):
                proj(CKV0 + j * 128, 128, bk[j])
                act(sq, ps[:, bk[j], :], AF.Square, [psr[bk[j]]], [res("sq")])
                mm(ps[:, sb_, :], ones[:], sq, j == 0, j == 1, [res("sq"), rc], [psr[sb_]])
                yield
            act(rstdb, ps[:, sb_, :], AF.Sqrt, [psr[sb_]], [res("rstdb")], scale=1.0 / 256, bias=EPS)
            recip(rstdb, rstdb, [res("rstdb")], [res("rstdb")])
            for j in range(2):
                stt(ckvn[:, j, :], ps[:, bk[j], :], gkv[:, j:j + 1], rstdb, ALU.mult, ALU.mult, [psr[bk[j]], res("rstdb"), rc], [res("ckvn")])
            dma("pool", ckvT_scr[:, :, t0:t0 + 512], ckvn, [res("ckvn")], [res("ckvT_scr")], "ckvs")
            b = gbank()
            proj(KRI0, 112, b)
            cp("act", xq[0:112, :], ps[0:112, b, :], [psr[b]], [res("xq")])
            dma("act", wiT_scr[:, t0:t0 + 512], xq[96:112, :], [res("xq")], [res("wiT_scr")], "wis")
            cp("act", xb[0:112, :], xq[0:112, :], [res("xq")], [res("xb")])
            b2 = gbank()
            mm(ps[0:112, b2, :], rm[0:112, 1, 0:112], xb[0:112, :], True, True, [res("xb"), rc], [psr[b2]])
            tt("pool", t1[0:112, :], xq[0:112, :], csb[0:112, 0, :], ALU.mult, [res("xq"), r_cs], [res("t1")])
            tt("dve", t2[0:112, :], ps[0:112, b2, :], csb[0:112, 1, :], ALU.mult, [psr[b2], r_cs], [res("t2")])
            tt("pool", krio[0:96, :], t1[0:96, :], t2[0:96, :], ALU.add, [res("t1"), res("t2")], [res("krio")])
            dma("pool", kri_scr[:, t0:t0 + 512], krio[0:96, :], [res("krio")], [res("kri_scr")], "kris")
            yield
            for h in range(8):
                b = gbank()
                mm(ps[:, b, :], WukT[:, 0, h, :], ckvn[:, 0, :], True, False, [r_W2, res("ckvn")], [psr[b]])
                mm(ps[:, b, :], WukT[:, 1, h, :], ckvn[:, 1, :], False, False, [r_W2, res("ckvn")], [psr[b]])
                mm(ps[:, b, :], Esel[0:96, :], krio[0:96, :], False, True, [r_W2, res("krio")], [psr[b]])
                cp("act" if h % 2 else "dve", KhTc[:, h, :], ps[:, b, :], [psr[b]], [res("KhTc")])
                yield
            dma("act", KhT_scr[:, :, t0:t0 + 512], KhTc, [res("KhTc")], [res("KhT_scr")], "khs")
            for q4 in range(4):
                for n2 in range(2):
                    b = gbank()
                    for j in range(2):
                        mm(ps[:, b, :], ckvn[:, j, q4 * 128:(q4 + 1) * 128], Wuv2[:, j].rearrange("p h v -> p (h v)")[:, n2 * 512:(n2 + 1) * 512], j == 0, j == 1, [res("ckvn"), r_W2], [psr[b]])
                    cp("act" if n2 else "dve", VHc[:, q4, n2 * 512:(n2 + 1) * 512], ps[:, b, :], [psr[b]], [res("VHc")])
                    yield
            for h in range(8):
                dma("act", VH_scr[h, :, 4 * c:4 * c + 4, :], VHc[:, :, h * 128:(h + 1) * 128], [res("VHc")], [res("VH_scr")], "vhs")
            for j in range(8 if c >= OC0 else 0):
                b = gbank()
                proj(GA0 + j * 128, 128, b)
                g = gao[j % 2]
                act(g, ps[:, b, :], AF.Silu, [psr[b]], [res("gao%d" % (j % 2))])
                dma("act", gaT_scr[:, j, t0:t0 + 512], g, [res("gao%d" % (j % 2))], [res("gaT_scr")], "gas%d" % (j % 2))
                yield

            yield

        def _drain(gen):
            for _ in gen:
                pass

        _drain(xproc_gen(0))
        for c in range(NCH):
            pg = proj_gen(c)
            xg = xproc_gen(c + 1) if c + 1 < NCH else None
            n_ = 0
            for _ in pg:
                n_ += 1
                if xg is not None and n_ % 3 == 0:
                    next(xg, None)
            if xg is not None:
                _drain(xg)
        S.barrier()

        AR.reset()
        kiT = AR.alloc([128, T], BF16)
        Wqi = AR.alloc([128, 4, 1024], BF16)
        cq2 = [AR.alloc([128, 4, 128], BF16) for _ in range(2)]
        cs2 = [AR.alloc([128, 2, 4, 128], F32) for _ in range(2)]
        wtok = [AR.alloc([128, 16], F32) for _ in range(2)]
        rcol = [AR.alloc([128, 2], F32) for _ in range(2)]
        wsc = [AR.alloc([128, 16], F32) for _ in range(2)]
        xq3 = [AR.alloc([128, 512], F32) for _ in range(2)]
        xb3 = [AR.alloc([128, 512], BF16) for _ in range(2)]
        t13 = [AR.alloc([128, 512], F32) for _ in range(2)]
        t23 = [AR.alloc([128, 512], F32) for _ in range(2)]
        qiT = [AR.alloc([128, 8, 128], BF16) for _ in range(2)]
        Dm = [AR.alloc([128, 16, 128], BF16) for _ in range(2)]
        NRL = 3
        rl = [AR.alloc([128, 2, 512], BF16) for _ in range(NRL)]
        isc = [AR.alloc([128, T], F32) for _ in range(2)]
        junk = AR.alloc([128, T], BF16)
        m01 = [AR.alloc([128, T], BF16) for _ in range(2)]
        mT = [AR.alloc([128, NTT, 128], BF16) for _ in range(2)]
        bs = [AR.alloc([128, 8], F32) for _ in range(2)]
        r_kiT, r_Wqi = res("kiT"), res("Wqi")
        dma("sp", kiT[0:64, :], kri_scr[32:96, :], [res("kri_scr")], [r_kiT], "p3Ksp")
        dma("sp", kiT[64:128, :], kri_scr[32:96, :], [res("kri_scr")], [r_kiT], "p3Ksp")
        dma("pool", Wqi, w_uqi.rearrange("(f p) n -> p f n", p=128), [], [r_Wqi], "p3Kpool")
        ISBS = [6, 7]
        isb_cnt = [0]
        rl_cnt = [0]
        pair_cnt = [0]
        LA = 2

        def p3_loads(qb):
            t0 = qb * 128
            s = qb % 2
            r_in = res("p3in%d" % s)
            dma("sp", cq2[s], cqg_scr[:, :, t0:t0 + 128], [res("cqg_scr")], [r_in], "p3in%d" % s)
            for s_ in range(2):
                dma("sp", cs2[s][:, s_, :, :], tabs[2, s_, :, t0:t0 + 128].unsqueeze(1).to_broadcast([128, 4, 128]), [], [r_in], "p3in%d" % s)
            dma("sp", wtok[s], wiT_scr[:, t0:t0 + 128].rearrange("h p -> p h"), [res("wiT_scr")], [r_in], "p3in%d" % s, slow=True)
            dma("sp", rcol[s][:, 0:1], rstdq_scr[0:1, t0:t0 + 128].rearrange("o p -> p o"), [res("rstdq_scr")], [r_in], "p3in%d" % s, slow=True)

        def p3_prologue(qb):
            s = qb % 2
            r_in = res("p3in%d" % s)
            r_q = res("qiT%d" % s)
            for half in range(2):
                hs = half
                b = gbank()
                for jj in range(4):
                    j = half * 4 + jj
                    for f in range(4):
                        mm(ps[:, b, jj * 128:(jj + 1) * 128], Wqi[:, f, j * 128:(j + 1) * 128], cq2[s][:, f, :], f == 0, f == 3, [r_Wqi, r_in], [psr[b]])
                cp("act", xb3[hs], ps[:, b, :], [psr[b]], [res("xb3%d" % hs)])
                cp("act", xq3[hs], ps[:, b, :], [psr[b]], [res("xq3%d" % hs)])
                b2 = gbank()
                mm(ps[:, b2, :], rm[:, 2, :], xb3[hs], True, True, [res("xb3%d" % hs), rc], [psr[b2]])
                tt("pool", t13[hs], xq3[hs], cs2[s][:, 0].rearrange("p a b -> p (a b)"), ALU.mult, [res("xq3%d" % hs), r_in], [res("t13%d" % hs)])
                tt("dve", t23[hs], ps[:, b2, :], cs2[s][:, 1].rearrange("p a b -> p (a b)"), ALU.mult, [psr[b2], r_in], [res("t23%d" % hs)])
                tt("pool", qiT[s][:, half * 4:half * 4 + 4, :].rearrange("p a b -> p (a b)"), t13[hs], t23[hs], ALU.add, [res("t13%d" % hs), res("t23%d" % hs)], [r_q])
            ts("pool", wsc[s], wtok[s], rcol[s][:, 0:1], 1.0 / 32, ALU.mult, ALU.mult, [r_in], [res("wsc%d" % s)])
            for h in range(16):
                ts("pool", Dm[s][:, h, :], ident[:], wsc[s][:, h:h + 1], 1.0, ALU.mult, ALU.mult, [rc, res("wsc%d" % s)], [res("Dm%d" % s)])

        def p3_groups(qb):
            s = qb % 2
            ng = qb // 4 + 1
            items = [(g, hp) for g in range(ng) for hp in range(8)]
            st = {}

            def stageA(g, hp):
                nt = min(4, qb + 1 - 4 * g)
                W = nt * 128
                k0 = g * 512
                pr = pair_cnt[0] % 3
                pair_cnt[0] += 1
                for e in range(2):
                    b = 2 * pr + e
                    mm(ps[:, b, 0:W], qiT[s][64 * e:64 * e + 64, hp, :], kiT[64 * e:64 * e + 64, k0:k0 + W], True, True, [res("qiT%d" % s), r_kiT], [psr[b]])
                ri = rl_cnt[0] % NRL
                rl_cnt[0] += 1
                act(rl[ri][:, :, 0:W], ps[:, 2 * pr:2 * pr + 2, 0:W], AF.Relu, [psr[2 * pr], psr[2 * pr + 1]], [res("rl%d" % ri)])
                st[(g, hp)] = ri

            def stageB(g, hp):
                nt = min(4, qb + 1 - 4 * g)
                W = nt * 128
                k0 = g * 512
                if hp == 0:
                    isb_cnt[0] += 1
                ISB = ISBS[isb_cnt[0] % 2]
                ri = st[(g, hp)]
                for e in range(2):
                    h = 2 * hp + e
                    mm(ps[:, ISB, 0:W], Dm[s][:, h, :], rl[ri][:, e, 0:W], h == 0, h == 15, [res("Dm%d" % s), res("rl%d" % ri)], [psr[ISB]])
                if hp == 7:
                    r_i = res("isc%d" % s)
                    if g == ng - 1:
                        cp("act", isc[s][:, k0:k0 + W], ps[:, ISB, 0:W], [psr[ISB]], [r_i])
                        tt("pool", isc[s][:, k0 + W - 128:k0 + W], isc[s][:, k0 + W - 128:k0 + W], cm[:], ALU.add, [r_i, rc], [r_i])
                    elif g < OC0:
                        act(isc[s][:, k0:k0 + W], ps[:, ISB, 0:W], AF.Identity, [psr[ISB], rc], [r_i], bias=padb[:, 0:1])
                    else:
                        cp("act", isc[s][:, k0:k0 + W], ps[:, ISB, 0:W], [psr[ISB]], [r_i])
            n = len(items)
            for i in range(n + LA):
                if i < n:
                    stageA(*items[i])
                if i - LA >= 0:
                    stageB(*items[i - LA])

        NIT3 = 15

        def p3_bisect(qb):
            s = qb % 2
            Sk = (qb + 1) * 128
            b_ = bs[s]
            r_i = res("isc%d" % s)
            r_bs = res("bs_%d" % s)
            memset("dve", b_[:, 0:1], 0.0, [r_bs])
            for it in range(NIT3):
                Wk = 16.0 / (2 ** it)
                ts("dve", junk[:, 0:Sk], isc[s][:, 0:Sk], b_[:, 0:1], None, ALU.is_ge, ALU.add, [r_i, r_bs], [res("junk"), res("bs2_%d" % s)], accum=b_[:, 2:3])
                ts("dve", b_[:, 3:4], b_[:, 2:3], 255.5, Wk, ALU.is_ge, ALU.mult, [res("bs2_%d" % s)], [res("bs3_%d" % s)])
                ts("dve", b_[:, 0:1], b_[:, 3:4], b_[:, 0:1], -Wk / 2, ALU.add, ALU.add, [r_bs, res("bs3_%d" % s)], [r_bs])
            ts("dve", b_[:, 1:2], b_[:, 0:1], -16.0 / (2 ** NIT3), None, ALU.add, None, [r_bs], [res("bs1_%d" % s)])
            ts("dve", m01[s][:, 0:Sk], isc[s][:, 0:Sk], b_[:, 1:2], None, ALU.is_ge, None, [r_i, res("bs1_%d" % s)], [res("m01_%d" % s)])

        def p3_transposes(qb):
            s = qb % 2
            mt = mT[s]
            r_mt = res("mT%d" % s)
            for g8 in range((qb + 8) // 8):
                n8 = min(8, qb + 1 - 8 * g8)
                b = gbank()
                pb = psb(b)
                for i in range(n8):
                    kt = g8 * 8 + i
                    tr(pb[:, i * 128:(i + 1) * 128], m01[s][:, kt * 128:(kt + 1) * 128], [res("m01_%d" % s), rc], [psr[b]])
                cp("act", mt[:, g8 * 8:g8 * 8 + n8, :].rearrange("p a b -> p (a b)"), pb[:, 0:n8 * 128], [psr[b]], [r_mt])
            c_, j_ = qb // 4, qb % 4
            nk_ = 4 * c_ + 4
            dma("act", maskT_scr[c_, :, 0:nk_, j_ * 128:(j_ + 1) * 128], mt[:, 0:nk_, :], [r_mt], [res("maskT_scr")], "mts%d" % s)

        memset("pool", mT[0][:], 0.0, [res("mT0")])
        memset("pool", mT[1][:], 0.0, [res("mT1")])
        p3_loads(OT0)
        p3_loads(OT0 + 1)
        p3_prologue(OT0)
        for qb in range(OT0, NTT):
            p3_groups(qb)
            if qb + 2 < NTT:
                p3_loads(qb + 2)
            if qb + 1 < NTT:
                p3_prologue(qb + 1)
            p3_bisect(qb)
            if qb >= OT0 + 1:
                p3_transposes(qb - 1)
        p3_transposes(NTT - 1)
        S.barrier()

        AR.reset()
        Wq = AR.alloc([128, 4, 1024], BF16)
        cq4 = AR.alloc([128, 4, 512], BF16)
        rs4 = AR.alloc([128, 512], F32)
        cs4 = AR.alloc([128, 2, 512], F32)
        mTc = AR.alloc([128, NTT, 512], BF16)
        ga4 = AR.alloc([128, 8, 512], BF16)
        xq4 = [AR.alloc([128, 512], F32) for _ in range(2)]
        xb4 = [AR.alloc([128, 512], BF16) for _ in range(2)]
        t14 = [AR.alloc([128, 512], F32) for _ in range(2)]
        t24 = [AR.alloc([128, 512], F32) for _ in range(2)]
        qh = AR.alloc([128, 8, 512], BF16)
        Kh = [AR.alloc([128, T], BF16) for _ in range(2)]
        VH = [AR.alloc([128, NTT, 128], BF16) for _ in range(2)]
        NPB = 3
        PT = [AR.alloc([128, 2, 512], BF16) for _ in range(NPB)]
        PmT = [AR.alloc([128, 2, 512], BF16) for _ in range(NPB)]
        rzb = [AR.alloc([128, 512], F32) for _ in range(2)]
        tmp4 = [AR.alloc([128, 512], F32) for _ in range(2)]
        oA = AR.alloc([128, 8, 512], BF16)
        r_K = res("Kres")
        dma("pool", Wq, w_uq.rearrange("(f p) n -> p f n", p=128), [], [r_K], "p4Kpool")
        ACCS = [[4, 5], [6, 7]]
        gb4 = [0]

        def gbank4():
            gb4[0] += 1
            return gb4[0] % 4
        pcnt = [0]
        hc4 = [0]
        LA4 = 2

        def p4_kv_load(c, h, slot):
            nkt = 4 * c + 4
            r_kv = res("KV%d" % slot)
            dma("sp", Kh[slot][:, 0:nkt * 128], KhT_scr[:, h, 0:nkt * 128], [res("KhT_scr")], [r_kv], "kv%d" % slot)
            dma("sp", VH[slot][:, 0:nkt, :], VH_scr[h, :, 0:nkt, :], [res("VH_scr")], [r_kv], "kv%d" % slot)

        heads_seq = [(c, h) for c in range(OC0, NCH) for h in range(8)]
        p4_kv_load(heads_seq[0][0], heads_seq[0][1], 0)
        for c in range(OC0, NCH):
            t0 = c * 512
            nkt = 4 * c + 4
            r_in = res("p4in")
            r_m = res("p4mask")
            dma("sp", cq4, cqg_scr[:, :, t0:t0 + 512], [res("cqg_scr")], [r_in], "p4in")
            dma("sp", rs4, rstdq_scr[0:1, t0:t0 + 512].partition_broadcast(128), [res("rstdq_scr")], [r_in], "p4in")
            dma("sp", cs4, tabs[0, :, :, t0:t0 + 512].rearrange("s p n -> p s n"), [], [r_in], "p4in")
            dma("sp", ga4, gaT_scr[:, :, t0:t0 + 512], [res("gaT_scr")], [r_in], "p4in")
            dma("sp", mTc[:, 0:nkt, :], maskT_scr[c, :, 0:nkt, :], [res("maskT_scr")], [r_m], "p4mask")
            r_qh = res("qh")
            pend = []
            for h in range(8):
                hs = h % 2
                b = gbank4()
                for f in range(4):
                    mm(ps[:, b, :], Wq[:, f, h * 128:(h + 1) * 128], cq4[:, f, :], f == 0, f == 3, [r_K, r_in], [psr[b]])
                for p_ in pend:
                    p_()
                pend = []
                tt("dve", xq4[hs], ps[:, b, :], rs4, ALU.mult, [psr[b], r_in], [res("xq4%d" % hs)])
                cp("act", xb4[hs], xq4[hs], [res("xq4%d" % hs)], [res("xb4%d" % hs)])

                def part2(h=h, hs=hs):
                    b2 = gbank4()
                    mm(ps[:, b2, :], rm[:, 0, :], xb4[hs], True, True, [res("xb4%d" % hs), rc], [psr[b2]])
                    tt("pool", t14[hs], xq4[hs], cs4[:, 0, :], ALU.mult, [res("xq4%d" % hs), r_in], [res("t14%d" % hs)])
                    tt("dve", t24[hs], ps[:, b2, :], cs4[:, 1, :], ALU.mult, [psr[b2], r_in], [res("t24%d" % hs)])
                    tt("pool", qh[:, h, :], t14[hs], t24[hs], ALU.add, [res("t14%d" % hs), res("t24%d" % hs)], [r_qh])
                pend.append(part2)
            for p_ in pend:
                p_()
            items = [(h, kp) for h in range(8) for kp in range(nkt // 2)]
            st4 = {}
            slot_of = {}

            def stageA(h, kp):
                if kp == 0:
                    slot_of[h] = hc4[0] % 2
                    hc4[0] += 1
                if kp == LA4:
                    idx = heads_seq.index((c, h))
                    if idx + 1 < len(heads_seq):
                        p4_kv_load(heads_seq[idx + 1][0], heads_seq[idx + 1][1], hc4[0] % 2)
                sl = slot_of[h]
                pr = 2 * (pcnt[0] % 2)
                for e in range(2):
                    kt = 2 * kp + e
                    mm(ps[:, pr + e, :], Kh[sl][:, kt * 128:(kt + 1) * 128], qh[:, h, :], True, True, [res("KV%d" % sl), r_qh], [psr[pr + e]])
                pi = pcnt[0] % NPB
                pcnt[0] += 1
                act(PT[pi], ps[:, pr:pr + 2, :], AF.Exp, [psr[pr], psr[pr + 1]], [res("PT%d" % pi)], scale=SCALE)
                tt("pool" if (kp < 2 or pcnt[0] % 4 == 0) else "dve", PmT[pi], PT[pi], mTc[:, 2 * kp:2 * kp + 2, :], ALU.mult, [res("PT%d" % pi), r_m], [res("PmT%d" % pi)])
                st4[(h, kp)] = pi

            def stageB(h, kp):
                pi = st4[(h, kp)]
                sl = slot_of[h]
                ACC = ACCS[sl]
                for e in range(2):
                    kt = 2 * kp + e
                    mm(ps[:, ACC[0], :], VH[sl][:, kt, :], PmT[pi][:, e, :], kt == 0, kt == nkt - 1, [res("KV%d" % sl), res("PmT%d" % pi)], [psr[ACC[0]]])
                for e in range(2):
                    kt = 2 * kp + e
                    mm(ps[:, ACC[1], :], ones[:], PmT[pi][:, e, :], kt == 0, kt == nkt - 1, [rc, res("PmT%d" % pi)], [psr[ACC[1]]])
                if kp == nkt // 2 - 1:
                    recip(rzb[sl], ps[:, ACC[1], :], [psr[ACC[1]]], [res("rzb%d" % sl)])
                    tt("dve", tmp4[sl], ps[:, ACC[0], :], rzb[sl], ALU.mult, [psr[ACC[0]], res("rzb%d" % sl)], [res("tmp4%d" % sl)])
                    tt("pool", oA[:, h, :], tmp4[sl], ga4[:, h, :], ALU.mult, [res("tmp4%d" % sl), r_in], [res("oA")])
            n = len(items)
            for i in range(n + LA4):
                if i < n:
                    stageA(*items[i])
                if i - LA4 >= 0:
                    stageB(*items[i - LA4])
            dma("pool", oT_scr[:, 0:8, t0:t0 + 512], oA, [res("oA")], [res("oT_scr")], "oas")
        S.barrier()

        AR.reset()
        HT = T // 2
        Wb = [AR.alloc([128, 16, 4, 128], BF16) for _ in range(2)]
        hT5 = [AR.alloc([128, 16, 512], BF16) for _ in range(2)]
        cs5 = [AR.alloc([128, 2, 512], F32) for _ in range(2)]
        qT = [AR.alloc([128, HT], BF16) for _ in range(2)]
        kT = [AR.alloc([128, T], BF16) for _ in range(2)]
        vT = [AR.alloc([128, T], BF16) for _ in range(2)]
        gT = [AR.alloc([128, HT], BF16) for _ in range(2)]
        VB = AR.alloc([128, 32, 128], BF16)
        xq5 = [AR.alloc([128, 512], F32) for _ in range(2)]
        xb5 = [AR.alloc([128, 512], BF16) for _ in range(2)]
        t15 = [AR.alloc([128, 512], F32) for _ in range(2)]
        t25 = [AR.alloc([128, 512], F32) for _ in range(2)]
        NP5 = 3
        PT5 = [AR.alloc([128, 512], BF16) for _ in range(NP5)]
        Pm5 = [AR.alloc([128, 512], BF16) for _ in range(NP5)]
        accO = AR.alloc([128, HT], F32)
        accZ = AR.alloc([128, HT], F32)
        oB = AR.alloc([128, HT], BF16)
        COLS = [1904, 2928, 3952, 4976]
        rcnt = [0]
        p5cnt = [0]
        LA5 = 2

        def load_wb(h):
            for i in range(4):
                dma("pool", Wb[h % 2][:, :, i, :], w_in[:, COLS[i] + 128 * h:COLS[i] + 128 * h + 128].rearrange("(kc p) n -> p kc n", p=128), [], [res("Wb%d" % (h % 2))], "wb%d" % (h % 2))

        def proj_gen(h):
            s5 = h % 2
            wbh = Wb[s5]
            r_wb = res("Wb%d" % s5)
            if h + 1 < 8:
                load_wb(h + 1)
            pend5 = []
            for c in range(NCH):
                t0 = c * 512
                hc = hT5[c % 2]
                rh = res("hT5_%d" % (c % 2))
                dma("sp", hc.rearrange("p a b -> p (a b)"), hT_scr[c], [res("hT_scr")], [rh], "h5_%d" % (c % 2))
                csb = cs5[c % 2]
                r_cs = res("cs5_%d" % (c % 2))
                dma("sp", csb, tabs[0, :, :, t0:t0 + 512].rearrange("s p n -> p s n"), [], [r_cs], "c5_%d" % (c % 2))
                for i in range(4):
                    if c < OC0 and i in (0, 3):
                        continue
                    b = gbank()
                    for kc in range(16):
                        mm(ps[:, b, :], wbh[:, kc, i, :], hc[:, kc, :], kc == 0, kc == 15, [r_wb, rh], [psr[b]])
                    for p_ in pend5:
                        p_()
                    pend5 = []
                    if i < 2:
                        if i == 0:
                            dst = qT[s5][:, t0 - HT:t0 - HT + 512]
                            r_dst = res("qT%d" % s5)
                        else:
                            dst = kT[s5][:, t0:t0 + 512]
                            r_dst = res("kT%d" % s5)
                        ri = rcnt[0] % 2
                        rcnt[0] += 1
                        cp("act", xb5[ri], ps[:, b, :], [psr[b]], [res("xb5%d" % ri)])
                        cp("act", xq5[ri], ps[:, b, :], [psr[b]], [res("xq5%d" % ri)])

                        def part2(ri=ri, dst=dst, csb=csb, r_cs=r_cs, r_dst=r_dst):
                            b2 = gbank()
                            mm(ps[:, b2, :], rm[:, 0, :], xb5[ri], True, True, [res("xb5%d" % ri), rc], [psr[b2]])
                            tt("pool", t15[ri], xq5[ri], csb[:, 0, :], ALU.mult, [res("xq5%d" % ri), r_cs], [res("t15%d" % ri)])
                            tt("dve", t25[ri], ps[:, b2, :], csb[:, 1, :], ALU.mult, [psr[b2], r_cs], [res("t25%d" % ri)])
                            tt("pool", dst, t15[ri], t25[ri], ALU.add, [res("t15%d" % ri), res("t25%d" % ri)], [r_dst])
                        pend5.append(part2)
                    elif i == 2:
                        cp("dve", vT[s5][:, t0:t0 + 512], ps[:, b, :], [psr[b]], [res("vT%d" % s5)])
                    else:
                        act(gT[s5][:, t0 - HT:t0 - HT + 512], ps[:, b, :], AF.Silu, [psr[b]], [res("gT%d" % s5)])
                    yield
            for p_ in pend5:
                p_()
            yield

        def attn_gen(h):
            s5 = h % 2
            r_q, r_k, r_v, r_g = res("qT%d" % s5), res("kT%d" % s5), res("vT%d" % s5), res("gT%d" % s5)
            first = True
            for d in (1, 4, 16):
                nblk = T // (128 * d)
                for g8 in range(4):
                    b = gbank()
                    pb = psb(b)
                    for i in range(8):
                        ti = g8 * 8 + i
                        r_, bl = ti // nblk, ti % nblk
                        st_ = r_ + d * 128 * bl
                        src = vT[s5][:, st_:st_ + 127 * d + 1:d]
                        tr(pb[:, i * 128:(i + 1) * 128], src, [r_v, rc], [psr[b]])
                    cp("act" if g8 % 2 else "dve", VB[:, g8 * 8:g8 * 8 + 8, :].rearrange("p a b -> p (a b)"), pb[:, 0:1024], [psr[b]], [res("VB")])
                    yield
                qtiles = [(r_, bl) for r_ in range(d) for bl in range(nblk // 2, nblk)]
                pairs = [qtiles[i:i + 2] for i in range(0, 16, 2)]
                stp = {}

                def stageA(pi_, pair):
                    b = gbank()
                    slots = []
                    for qi_, (r_, bl) in enumerate(pair):
                        st_q = r_ + d * 128 * bl - HT
                        qap = qT[s5][:, st_q:st_q + 127 * d + 1:d]
                        for kk in range(2):
                            kb_ = bl - 1 + kk
                            sl = qi_ * 2 + kk
                            st_k = r_ + d * 128 * kb_
                            kap = kT[s5][:, st_k:st_k + 127 * d + 1:d]
                            mm(ps[:, b, sl * 128:(sl + 1) * 128], kap, qap, True, True, [r_k, r_q], [psr[b]])
                            slots.append((sl, qi_, r_ * nblk + kb_))
                    pp = p5cnt[0] % NP5
                    p5cnt[0] += 1
                    act(PT5[pp], ps[:, b, :], AF.Exp, [psr[b]], [res("PT5%d" % pp)], scale=SCALE)
                    npad = sum(1 for (r_, bl) in pair if bl == nblk // 2)
                    bsel = 0 if npad == 0 else (2 if npad == 2 else 1)
                    assert npad != 1 or pair[0][1] == nblk // 2
                    tt("pool" if pp % 2 else "dve", Pm5[pp], PT5[pp], band[:, bsel, :], ALU.mult, [res("PT5%d" % pp), rc], [res("Pm5%d" % pp)])
                    stp[pi_] = (pp, slots)

                def stageB(pi_, pair, first=first):
                    pp, slots = stp[pi_]
                    b3 = gbank()
                    for qi_, (r_, bl) in enumerate(pair):
                        mine = [s_ for s_ in slots if s_[1] == qi_]
                        for n_, (sl, _, vt_) in enumerate(mine):
                            mm(ps[:, b3, qi_ * 128:(qi_ + 1) * 128], VB[:, vt_, :], Pm5[pp][:, sl * 128:(sl + 1) * 128], n_ == 0, n_ == len(mine) - 1, [res("VB"), res("Pm5%d" % pp)], [psr[b3]])
                        for n_, (sl, _, vt_) in enumerate(mine):
                            mm(ps[:, b3, 256 + qi_ * 128:256 + (qi_ + 1) * 128], ones[:], Pm5[pp][:, sl * 128:(sl + 1) * 128], n_ == 0, n_ == len(mine) - 1, [rc, res("Pm5%d" % pp)], [psr[b3]])
                    for qi_, (r_, bl) in enumerate(pair):
                        st_q = r_ + d * 128 * bl - HT
                        sl_o = slice(st_q, st_q + 127 * d + 1, d)
                        if first:
                            cp("act", accO[:, sl_o], ps[:, b3, qi_ * 128:(qi_ + 1) * 128], [psr[b3]], [res("accO")])
                            cp("act", accZ[:, sl_o], ps[:, b3, 256 + qi_ * 128:256 + (qi_ + 1) * 128], [psr[b3]], [res("accZ")])
                        else:
                            tt("dve", accO[:, sl_o], accO[:, sl_o], ps[:, b3, qi_ * 128:(qi_ + 1) * 128], ALU.add, [psr[b3], res("accO")], [res("accO")])
                            tt("dve", accZ[:, sl_o], accZ[:, sl_o], ps[:, b3, 256 + qi_ * 128:256 + (qi_ + 1) * 128], ALU.add, [psr[b3], res("accZ")], [res("accZ")])
                n = len(pairs)
                for i in range(n + LA5):
                    if i < n:
                        stageA(i, pairs[i])
                    if i - LA5 >= 0:
                        stageB(i - LA5, pairs[i - LA5])
                    yield
                first = False
            for c4 in range(4):
                sl_ = slice(c4 * 512, (c4 + 1) * 512)
                recip(accZ[:, sl_], accZ[:, sl_], [res("accZ")], [res("accZ")])
                tt("dve", accO[:, sl_], accO[:, sl_], accZ[:, sl_], ALU.mult, [res("accO"), res("accZ")], [res("accO")])
                tt("pool", oB[:, sl_], accO[:, sl_], gT[s5][:, sl_], ALU.mult, [res("accO"), r_g], [res("oB")])
                yield
            dma("pool", oT_scr[:, 8 + h, HT:T], oB, [res("oB")], [res("oT_scr")], "obs")
            yield

        def drain(gen):
            for _ in gen:
                pass

        load_wb(0)
        drain(proj_gen(0))
        for h in range(8):
            ag = attn_gen(h)
            if h + 1 < 8:
                pg = proj_gen(h + 1)
                for _ in pg:
                    next(ag, None)
                    next(ag, None)
            drain(ag)
        S.barrier()

        AR.reset()
        Wo = AR.alloc([128, 16, D], BF16)
        oT6 = [AR.alloc([128, 16, 128], BF16) for _ in range(2)]
        x6 = [AR.alloc([128, D], F32) for _ in range(2)]
        y6 = [AR.alloc([128, D], F32) for _ in range(2)]
        jk6 = AR.alloc([128, 512], BF16)
        s6 = AR.alloc([128, 16], F32)
        r_Wo = res("Wo")
        for i in range(4):
            dma("pool", Wo[:, :, i * 512:(i + 1) * 512], w_out[:, i * 512:(i + 1) * 512].rearrange("(kc p) n -> p kc n", p=128), [], [r_Wo], "wo")
        fin = []
        for ti in range(OT0, NTT):
            t0 = ti * 128
            s = ti % 2
            YB = [4, 5, 6, 7] if s else [0, 1, 2, 3]
            r_in = res("p6in%d" % s)
            dma("sp", oT6[s], oT_scr[:, :, t0:t0 + 128], [res("oT_scr")], [r_in], "p6a%d" % s)
            dma("sp", x6[s], x[t0:t0 + 128, :], [], [res("x6_%d" % s)], "p6b%d" % s)
            for n in range(4):
                for kc in range(16):
                    mm(ps[:, YB[n], :], oT6[s][:, kc, :], Wo[:, kc, n * 512:(n + 1) * 512], kc == 0, kc == 15, [r_in, r_Wo], [psr[YB[n]]])
                act(jk6, ps[:, YB[n], :], AF.Square, [psr[YB[n]]], [res("jk6"), res("s6_%d" % n)], accum=s6[:, n:n + 1])
            tt("dve", s6[:, 4:5], s6[:, 0:1], s6[:, 1:2], ALU.add, [res("s6_0"), res("s6_1")], [res("s6a")])
            tt("dve", s6[:, 5:6], s6[:, 2:3], s6[:, 3:4], ALU.add, [res("s6_2"), res("s6_3")], [res("s6b")])
            tt("dve", s6[:, 6:7], s6[:, 4:5], s6[:, 5:6], ALU.add, [res("s6a"), res("s6b")], [res("s6c")])
            act(s6[:, 7:8], s6[:, 6:7], AF.Sqrt, [res("s6c")], [res("s6d")], scale=1.0 / D, bias=EPS)
            recip(s6[:, 8:9], s6[:, 7:8], [res("s6d")], [res("s6e")])
            yy = y6[s]
            r_yy = res("y6_%d" % s)
            for n in range(4):
                sl_ = slice(n * 512, (n + 1) * 512)
                stt(yy[:, sl_], ps[:, YB[n], :], s6[:, 8:9], Gbc[:, sl_], ALU.mult, ALU.mult, [psr[YB[n]], res("s6e"), res("Gbc")], [r_yy])
                tt("pool", yy[:, sl_], yy[:, sl_], x6[s][:, sl_], ALU.add, [r_yy, res("x6_%d" % s)], [r_yy])
            fin.append(dma("pool", out[t0 - OT0 * 128:t0 - OT0 * 128 + 128, :], yy, [r_yy], [res("out")], "outs%d" % s))
        S.emit(nc, final_waits=fin[-2:])
    return nc


def _host_inputs(inp):
    ident = np.eye(128, dtype=np.float32)
    f = lambda a: np.ascontiguousarray(np.asarray(a, dtype=np.float32))
    shared = {
        "w_ada": f(inp["w_ada"][0]),
        "bada_col": f(np.asarray(inp["b_ada"][0])[:4096].reshape(32, 128).T),
        "bada_g": f(np.asarray(inp["b_ada"][0])[4096:].reshape(1, D)),
        "gpre_col": f(np.asarray(inp["g_pre"][0]).reshape(16, 128).T),
        "gpost": f(np.asarray(inp["g_post"][0]).reshape(1, D)),
        "gq_col": f(np.asarray(inp["g_q"][0]).reshape(4, 128).T),
        "gkv_col": f(np.asarray(inp["g_kv"][0]).reshape(2, 128).T),
        "w_in": f(inp["w_in"][0]),
        "w_uq": f(inp["w_uq"][0]),
        "w_uqi": f(inp["w_uq_idx"][0]),
        "w_uk": f(inp["w_uk"][0]),
        "w_uv": f(inp["w_uv"][0]),
        "w_out": f(inp["w_out"][0]),
        "ident": ident,
    }
    per_g = []
    for g in range(2):
        tabs, rmats = _rope_tables(g)
        band, cm = _masks(g)
        padb = np.full((128, 512), 0.0 if g == 1 else NEG, np.float32)
        per_g.append({"tabs": tabs, "rmats": rmats, "band": f(band), "cm": f(cm), "padb": padb})
    maps = []
    xx = np.asarray(inp["x"], dtype=np.float32)
    cc = np.asarray(inp["c"], dtype=np.float32)
    for b in range(4):
        for g in range(2):
            m = dict(shared)
            m.update(per_g[g])
            if g == 1:
                m["x"] = np.ascontiguousarray(xx[b])
            else:
                xw = np.zeros((T, D), np.float32)
                xw[T // 2:] = xx[b, :T // 2]
                m["x"] = xw
            m["scol"] = np.ascontiguousarray(cc[b].reshape(16, 128).T)
            maps.append(m)
    return maps


_NC = None


def kernel(**inputs):
    global _NC
    if _NC is None:
        _NC = build()
    maps = _host_inputs(inputs)
    res_ = run_bass_kernel_spmd(_NC, maps, core_ids=list(range(8)))
    out = np.empty((4, T, D), np.float32)
    for b in range(4):
        for g in range(2):
            out[b, g * (T // 2):(g + 1) * (T // 2)] = np.asarray(res_.results[2 * b + g]["out"], dtype=np.float32)
    return out
```

```python
import contextlib
import numpy as np
import concourse.bass as bass
import concourse.mybir as mybir
from concourse.bass_utils import run_bass_kernel_spmd

F32 = mybir.dt.float32
BF16 = mybir.dt.bfloat16
AF = mybir.ActivationFunctionType
ALU = mybir.AluOpType
AX = mybir.AxisListType

T = 4096
D = 2048
KC = 16
NCH = 8
NTT = 32
EPS = 1e-6
THETA = 500000.0
NIT = 20
OT0 = 16
OC0 = 4
NEG = -1.0e30
SCALE = 128.0 ** -0.5


class Res:
    __slots__ = ("name", "writers", "readers")

    def __init__(self, name):
        self.name = name
        self.writers = {}
        self.readers = []


class Op:
    __slots__ = ("eng", "fn", "deps", "users", "sem", "val", "is_dma", "slot")

    def __init__(self, eng, fn):
        self.eng = eng
        self.fn = fn
        self.deps = []
        self.users = 0
        self.sem = None
        self.val = 0
        self.is_dma = False
        self.slot = None


class Sched:
    ENGS = ("pe", "act", "dve", "pool", "sp")

    def __init__(self):
        self.ops = {e: [] for e in self.ENGS}
        self.slots = {}
        self.last = {e: None for e in self.ENGS}
        self.pending_dma = []
        self.slotinfo = {}

    def add(self, eng, fn, reads=(), writes=(), dma_slot=None):
        op = Op(eng, fn)
        raw = []
        oth = []
        for r in reads:
            raw.extend(r.writers.values())
        for w in writes:
            if eng != "pe":
                raw.extend(w.writers.values())
            else:
                oth.extend(w.writers.values())
            oth.extend(w.readers)
        seen = set()
        for d in raw:
            if id(d) in seen or d is op:
                continue
            if d.eng == eng and not d.is_dma and eng == "pe":
                continue
            seen.add(id(d))
            op.deps.append(d)
        for d in oth:
            if id(d) in seen or d is op:
                continue
            if d.eng == eng and not d.is_dma and eng != "pool" and dma_slot is None:
                continue
            seen.add(id(d))
            op.deps.append(d)
        ups = []
        seen2 = set()
        for d in op.deps:
            if d.is_dma:
                info = self.slotinfo[d.slot]
                d = info["last"]
                info["waited"] = d
            if id(d) not in seen2:
                seen2.add(id(d))
                ups.append(d)
        op.deps = ups
        if dma_slot is not None:
            op.is_dma = True
            op.slot = dma_slot
            info = self.slotinfo.setdefault(dma_slot, {"last": None, "waited": None})
            w_ = info["waited"]
            if w_ is not None and id(w_) not in seen2:
                op.deps.append(w_)
            info["last"] = op
            self.pending_dma.append(op)
        for d in op.deps:
            d.users += 1
        wkey = dma_slot if dma_slot is not None else eng
        for w in writes:
            w.writers[wkey] = op
            w.readers = []
        for r in reads:
            r.readers.append(op)
        self.ops[eng].append(op)
        self.last[eng] = op
        return op

    def barrier(self):
        lasts = [self.last[e] for e in self.ENGS if self.last[e] is not None]
        dmas = []
        for d in self.pending_dma:
            info = self.slotinfo[d.slot]
            d2 = info["last"]
            info["waited"] = d2
            if all(d2 is not x for x in dmas):
                dmas.append(d2)
        self.pending_dma = []
        for e in self.ENGS:
            op = Op(e, lambda eng: eng.nop())
            for d in lasts + dmas:
                if d.eng == e and not d.is_dma and e != "pool":
                    continue
                op.deps.append(d)
                d.users += 1
            self.ops[e].append(op)
            self.last[e] = op

    def emit(self, nc, final_waits=()):
        with contextlib.ExitStack() as st:
            esem = {e: st.enter_context(nc.semaphore("s_" + e)) for e in self.ENGS}
            for e in self.ENGS:
                for op in self.ops[e]:
                    if op.is_dma and op.slot not in self.slots:
                        self.slots[op.slot] = [st.enter_context(nc.semaphore("d_%d" % len(self.slots))), 0]
            for e in self.ENGS:
                cnt = 0
                for op in self.ops[e]:
                    if op.is_dma:
                        s = self.slots[op.slot]
                        s[1] += 16
                        op.sem = s[0]
                        op.val = s[1]
                    elif op.users > 0:
                        cnt += 1
                        op.sem = esem[e]
                        op.val = cnt
            block = st.enter_context(nc.Block())

            def run(e, engine):
                seen = {}
                for op in self.ops[e]:
                    for d in op.deps:
                        k = id(d.sem)
                        if seen.get(k, 0) >= d.val:
                            continue
                        engine.wait_ge(d.sem, d.val)
                        seen[k] = d.val
                    inst = op.fn(engine)
                    if op.is_dma:
                        inst.then_inc(op.sem, 16)
                    elif op.users > 0:
                        inst.then_inc(op.sem, 1)
                if e == "sp":
                    for d in final_waits:
                        k = id(d.sem)
                        if seen.get(k, 0) >= d.val:
                            continue
                        engine.wait_ge(d.sem, d.val)
                        seen[k] = d.val

            @block.tensor
            def _(eng):
                run("pe", eng)

            @block.scalar
            def _(eng):
                run("act", eng)

            @block.vector
            def _(eng):
                run("dve", eng)

            @block.gpsimd
            def _(eng):
                run("pool", eng)

            @block.sync
            def _(eng):
                run("sp", eng)


def _rope_tables(g):
    pos = np.maximum(np.arange(T, dtype=np.float32) + 2048.0 * (g - 1), 0.0).astype(np.float32)
    inv32 = (THETA ** (-np.arange(0, 32, 2, dtype=np.float32) / 32)).astype(np.float32)
    inv16 = (THETA ** (-np.arange(0, 16, 2, dtype=np.float32) / 16)).astype(np.float32)
    a32 = (pos[None, :] * inv32[:, None]).astype(np.float32)
    a16 = (pos[None, :] * inv16[:, None]).astype(np.float32)
    tabs = np.zeros((3, 2, 128, T), np.float32)
    tabs[:, 0] = 1.0
    tabs[0, 0, 0:16] = np.cos(a32); tabs[0, 0, 16:32] = np.cos(a32)
    tabs[0, 1, 0:16] = np.sin(a32); tabs[0, 1, 16:32] = np.sin(a32)
    tabs[1, 0, 0:32] = tabs[0, 0, 0:32]; tabs[1, 1, 0:32] = tabs[0, 1, 0:32]
    tabs[1, 0, 32:40] = np.cos(a16); tabs[1, 0, 40:48] = np.cos(a16)
    tabs[1, 1, 32:40] = np.sin(a16); tabs[1, 1, 40:48] = np.sin(a16)
    for b in (0, 64):
        tabs[2, 0, b:b + 8] = np.cos(a16); tabs[2, 0, b + 8:b + 16] = np.cos(a16)
        tabs[2, 1, b:b + 8] = np.sin(a16); tabs[2, 1, b + 8:b + 16] = np.sin(a16)
    rm = np.zeros((3, 128, 128), np.float32)

    def blk(R, base, half):
        for j in range(half):
            R[base + j + half, base + j] = -1.0
            R[base + j, base + j + half] = 1.0
    blk(rm[0], 0, 16)
    blk(rm[1], 0, 16); blk(rm[1], 32, 8)
    blk(rm[2], 0, 8); blk(rm[2], 64, 8)
    return tabs, rm


def _masks(g):
    k = np.arange(128)[:, None]
    q = np.arange(128)[None, :]
    prev = (k >= q).astype(np.float32)
    cur = (k <= q).astype(np.float32)
    pp = prev * float(g)
    band = np.stack([np.concatenate([prev, cur, prev, cur], axis=1),
                     np.concatenate([pp, cur, prev, cur], axis=1),
                     np.concatenate([pp, cur, pp, cur], axis=1)], axis=0)
    cm = np.where(np.arange(128)[None, :] <= np.arange(128)[:, None], 0.0, NEG).astype(np.float32)
    return band, cm


def build(debug=False):
    nc = bass.Bass("TRN2", target_bir_lowering=False)
    S = Sched()
    okind = "ExternalOutput" if debug else "Internal"

    def din(name, shape, dt=F32):
        return nc.dram_tensor(name, shape, dt, kind="ExternalInput").ap()

    def dscr(name, shape, dt):
        return nc.dram_tensor(name, shape, dt, kind=okind).ap()

    x = din("x", [T, D])
    scol = din("scol", [128, 16])
    w_ada = din("w_ada", [D, 3 * D])
    bada_col = din("bada_col", [128, 32])
    bada_g = din("bada_g", [1, D])
    gpre_col = din("gpre_col", [128, 16])
    gpost = din("gpost", [1, D])
    gq_col = din("gq_col", [128, 4])
    gkv_col = din("gkv_col", [128, 2])
    w_in = din("w_in", [D, 6000])
    w_uq = din("w_uq", [512, 1024])
    w_uqi = din("w_uqi", [512, 1024])
    w_uk = din("w_uk", [8, 96, 256])
    w_uv = din("w_uv", [8, 256, 128])
    w_out = din("w_out", [D, D])
    tabs = din("tabs", [3, 2, 128, T])
    rmats = din("rmats", [3, 128, 128])
    ident_d = din("ident", [128, 128])
    band_d = din("band", [3, 128, 512])
    padb_d = din("padb", [128, 512])
    cm_d = din("cm", [128, 128])
    out = nc.dram_tensor("out", [T // 2, D], F32, kind="ExternalOutput").ap()

    hT_scr = dscr("hT_scr", [NCH, 128, KC * 512], BF16)
    cqg_scr = dscr("cqg_scr", [128, 4, T], BF16)
    rstdq_scr = dscr("rstdq_scr", [1, T], F32)
    ckvT_scr = dscr("ckvT_scr", [128, 2, T], BF16)
    KhT_scr = dscr("KhT_scr", [128, 8, T], BF16)
    VH_scr = dscr("VH_scr", [8, 128, NTT, 128], BF16)
    kri_scr = dscr("kri_scr", [96, T], BF16)
    wiT_scr = dscr("wiT_scr", [16, T], F32)
    gaT_scr = dscr("gaT_scr", [128, 8, T], BF16)
    maskT_scr = dscr("maskT_scr", [NCH, 128, NTT, 512], BF16)
    oT_scr = dscr("oT_scr", [128, 16, T], BF16)

    R = {}

    def res(name):
        if name not in R:
            R[name] = Res(name)
        return R[name]

    with contextlib.ExitStack() as st:
        ps = st.enter_context(nc.psum_tensor("ps", [128, 8, 512], F32))
        psr = [res("psb%d" % i) for i in range(8)]
        gb = [0]

        def gbank(n=5):
            b = gb[0] % n
            gb[0] += 1
            return b

        def psb(b):
            return ps[:, b, :].bitcast(BF16)

        def sbp(name, shape, dt):
            return st.enter_context(nc.sbuf_tensor("sb_" + name, shape, dt))

        ident = sbp("identb", [128, 128], BF16)
        identf = sbp("identf", [128, 128], F32)
        ones = sbp("onesb", [128, 128], BF16)
        rm = sbp("rm", [128, 3, 128], BF16)
        band = sbp("bandb", [128, 3, 512], BF16)
        padb = sbp("padb", [128, 512], F32)
        cm = sbp("cm", [128, 128], F32)
        A1 = sbp("A1", [128, 16], F32)
        B1 = sbp("B1", [128, 16], F32)
        Gbc = sbp("Gbc", [128, D], F32)
        gq = sbp("gq", [128, 4], F32)
        gkv = sbp("gkv", [128, 2], F32)
        arena = sbp("arena", [128, 88576], BF16)

        class Arena:
            def __init__(self):
                self.off = 0

            def reset(self):
                self.off = 0

            def alloc(self, shape, dt):
                n = int(np.prod(shape[1:]))
                nb = n * (4 if dt == F32 else 2)
                nb = (nb + 63) // 64 * 64
                o = self.off
                self.off += nb // 2
                assert self.off <= 88576, self.off
                v = arena[0:shape[0], o:o + nb // 2]
                if dt == F32:
                    v = v.bitcast(F32)[:, 0:n]
                else:
                    v = v[:, 0:n]
                if len(shape) == 3:
                    v = v.rearrange("p (a b) -> p a b", a=shape[1])
                elif len(shape) == 4:
                    v = v.rearrange("p (a b c) -> p a b c", a=shape[1], b=shape[2])
                return v

        AR = Arena()

        def dma(eng, o, i, reads, writes, slot, slow=False):
            if slow:
                return S.add(eng, lambda e: e.dma_start(out=o, in_=i, allow_slow_non_contiguous=True), reads, writes, dma_slot=slot)
            return S.add(eng, lambda e: e.dma_start(out=o, in_=i), reads, writes, dma_slot=slot)

        def mm(o, l, r, start, stop, reads, writes):
            return S.add("pe", lambda e: e.matmul(o, lhsT=l, rhs=r, start=start, stop=stop), reads, writes)

        def tr(o, i, reads, writes, idt=None):
            idt = ident[:] if idt is None else idt
            return S.add("pe", lambda e: e.transpose(o, i, idt), reads, writes)

        def act(o, i, func, reads, writes, scale=1.0, bias=0.0, accum=None):
            if accum is None:
                return S.add("act", lambda e: e.activation(out=o, in_=i, func=func, scale=scale, bias=bias), reads, writes)
            return S.add("act", lambda e: e.activation(out=o, in_=i, func=func, scale=scale, bias=bias, accum_out=accum), reads, writes)

        def tt(eng, o, a, b, op, reads, writes):
            return S.add(eng, lambda e: e.tensor_tensor(out=o, in0=a, in1=b, op=op), reads, writes)

        def ts(eng, o, a, s1, s2, op0, op1, reads, writes, accum=None):
            if accum is None:
                if s2 is None:
                    return S.add(eng, lambda e: e.tensor_scalar(out=o, in0=a, scalar1=s1, scalar2=None, op0=op0), reads, writes)
                return S.add(eng, lambda e: e.tensor_scalar(out=o, in0=a, scalar1=s1, scalar2=s2, op0=op0, op1=op1), reads, writes)
            return S.add(eng, lambda e: e.tensor_scalar(out=o, in0=a, scalar1=s1, scalar2=s2, op0=op0, op1=op1, accum_out=accum), reads, writes)

        def stt(o, a, s, b, op0, op1, reads, writes):
            return S.add("dve", lambda e: e.scalar_tensor_tensor(out=o, in0=a, scalar=s, in1=b, op0=op0, op1=op1), reads, writes)

        def recip(o, i, reads, writes):
            return S.add("dve", lambda e: e.reciprocal(out=o, in_=i), reads, writes)

        def cp(eng, o, i, reads, writes):
            if eng == "act":
                return S.add("act", lambda e: e.copy(out=o, in_=i), reads, writes)
            return S.add(eng, lambda e: e.tensor_copy(out=o, in_=i), reads, writes)

        def memset(eng, o, v, writes):
            return S.add(eng, lambda e: e.memset(o, v), (), writes)

        rc = res("consts")
        dma("pool", ident[:], ident_d, [], [rc], "cpool")
        dma("sp", identf[:], ident_d, [], [rc], "csp")
        dma("pool", rm[:], rmats.rearrange("t p m -> p t m"), [], [rc], "cpool")
        dma("pool", band[:], band_d.rearrange("t p n -> p t n"), [], [rc], "cpool")
        dma("sp", padb[:], padb_d, [], [rc], "csp")
        dma("sp", cm[:], cm_d, [], [rc], "csp")
        dma("sp", gq[:], gq_col, [], [rc], "csp")
        dma("sp", gkv[:], gkv_col, [], [rc], "csp")
        memset("dve", ones[:], 1.0, [rc])

        AR.reset()
        sc = AR.alloc([128, 16], F32)
        scb = AR.alloc([128, 16], BF16)
        screp = AR.alloc([128, 16, 128], BF16)
        bcol = AR.alloc([128, 32], F32)
        gpc = AR.alloc([128, 16], F32)
        modc = AR.alloc([128, 32], F32)
        bg = AR.alloc([128, D], F32)
        gp = AR.alloc([128, D], F32)
        wblk = [AR.alloc([128, 16, 512], F32) for _ in range(2)]
        wbb = [AR.alloc([128, 16, 512], BF16) for _ in range(2)]
        r_sc, r_mod, r_bg = res("sc"), res("modc"), res("bg")
        r_wblk = [res("wblk0"), res("wblk1")]
        r_wbb = [res("wbb0"), res("wbb1")]
        dma("sp", sc, scol, [], [r_sc], "p0")
        dma("sp", bcol, bada_col, [], [r_sc], "p0")
        dma("sp", gpc, gpre_col, [], [r_sc], "p0")
        dma("sp", bg, bada_g.partition_broadcast(128), [], [r_bg], "p0")
        dma("sp", gp, gpost.partition_broadcast(128), [], [r_bg], "p0")
        act(sc, sc, AF.Silu, [r_sc], [r_sc])
        cp("dve", scb, sc, [r_sc], [r_sc])
        for kc in range(16):
            ts("dve", screp[:, kc, :], ones[:], sc[:, kc:kc + 1], None, ALU.mult, None, [r_sc, rc], [res("screp")])
        mb = 7
        CE = ["dve", "act", "pool", "dve"]
        for blk in range(12):
            wf = wblk[blk % 2]
            rwf = r_wblk[blk % 2]
            wb = wbb[blk % 2]
            rw = r_wbb[blk % 2]
            dma("sp" if blk % 2 == 0 else "act", wf, w_ada[:, blk * 512:(blk + 1) * 512].rearrange("(kc p) n -> p kc n", p=128), [], [rwf], "wblk%d" % (blk % 2))
            for q_ in range(4):
                cp(CE[q_], wb[:, q_ * 4:q_ * 4 + 4, :], wf[:, q_ * 4:q_ * 4 + 4, :], [rwf], [rw])
            if blk < 8:
                for jj in range(4):
                    j = blk * 4 + jj
                    for kc in range(16):
                        mm(ps[:, mb, j:j + 1], wb[:, kc, jj * 128:(jj + 1) * 128], scb[:, kc:kc + 1], kc == 0, kc == 15, [rw, r_sc], [psr[mb]])
            else:
                b = gbank()
                for kc in range(16):
                    mm(ps[:, b, :], screp[:, kc, :], wb[:, kc, :], kc == 0, kc == 15, [rw, res("screp")], [psr[b]])
                c0 = (blk - 8) * 512
                tt("dve", Gbc[:, c0:c0 + 512], ps[:, b, :], bg[:, c0:c0 + 512], ALU.add, [psr[b], r_bg], [res("Gbc")])
                tt("pool", Gbc[:, c0:c0 + 512], Gbc[:, c0:c0 + 512], gp[:, c0:c0 + 512], ALU.mult, [res("Gbc"), r_bg], [res("Gbc")])
        tt("dve", modc, ps[:, mb, 0:32], bcol, ALU.add, [psr[mb], r_sc], [r_mod])
        cp("dve", B1[:], modc[:, 0:16], [r_mod], [res("AB")])
        ts("dve", modc[:, 16:32], modc[:, 16:32], 1.0, None, ALU.add, None, [r_mod], [r_mod])
        tt("dve", A1[:], modc[:, 16:32], gpc, ALU.mult, [r_mod, r_sc], [res("AB")])
        S.barrier()

        AR.reset()
        Wa = AR.alloc([128, 16, 1904], BF16)
        xt = [AR.alloc([128, D], F32) for _ in range(2)]
        xn = AR.alloc([128, D], BF16)
        hTc = [AR.alloc([128, 16, 512], BF16) for _ in range(2)]
        st1 = AR.alloc([128, 8], F32)
        sq = AR.alloc([128, 512], BF16)
        rstdb = AR.alloc([128, 512], F32)
        cqg = AR.alloc([128, 4, 512], BF16)
        ckvn = AR.alloc([128, 2, 512], BF16)
        WukT = AR.alloc([128, 2, 8, 128], BF16)
        Wukn = AR.alloc([128, 8, 256], BF16)
        Wuv2 = AR.alloc([128, 2, 8, 128], BF16)
        Esel = AR.alloc([128, 128], BF16)
        KhTc = AR.alloc([128, 8, 512], BF16)
        VHc = AR.alloc([128, 4, 1024], BF16)
        xq = AR.alloc([128, 512], F32)
        xb = AR.alloc([128, 512], BF16)
        t1 = AR.alloc([128, 512], F32)
        t2 = AR.alloc([128, 512], F32)
        krio = AR.alloc([128, 512], BF16)
        gao = [AR.alloc([128, 512], BF16) for _ in range(2)]
        cst = [AR.alloc([128, 2, 512], F32) for _ in range(2)]
        r_Wa = res("Wa")
        r_xt = [res("xt0"), res("xt1")]
        r_hTc = [res("hTc0"), res("hTc1")]
        for i in range(4):
            dma("pool", Wa[:, :, i * 476:(i + 1) * 476], w_in[:, i * 476:(i + 1) * 476].rearrange("(kc p) n -> p kc n", p=128), [], [r_Wa], "Wa")
        CQ0, CKV0, KRI0, GA0 = 0, 512, 768, 880
        r_W2 = res("W2")
        dma("pool", Wukn[0:96, :, :], w_uk.rearrange("h n r -> n h r"), [], [res("Wukn")], "W2")
        for j in range(2):
            dma("pool", Wuv2[:, j], w_uv[:, j * 128:(j + 1) * 128, :].rearrange("h p v -> p h v"), [], [r_W2], "W2")
        memset("pool", WukT, 0.0, [r_W2])
        memset("pool", Esel, 0.0, [r_W2])
        cp("pool", Esel[0:32, 0:32], ident[0:32, 0:32], [rc], [r_W2])
        for j in range(2):
            b = gbank()
            pb = psb(b)
            for h in range(8):
                tr(pb[:, h * 96:(h + 1) * 96], Wukn[0:96, h, j * 128:(j + 1) * 128], [res("Wukn"), rc], [psr[b]], idt=ident[0:96, 0:96])
            cp("dve", WukT[:, j, :, 32:128], pb[:, 0:768].rearrange("p (h n) -> p h n", h=8), [psr[b]], [r_W2])

        def rope(src_f32, src_res, typ, cos, sin, cs_res, out_bf, out_res, nrows=128):
            n = src_f32.shape[-1]
            cp("act", xb[:, 0:n], src_f32, [src_res], [res("xb")])
            b = gbank()
            mm(ps[:, b, 0:n], rm[:, typ, :], xb[:, 0:n], True, True, [res("xb"), rc], [psr[b]])
            tt("pool", t1[:, 0:n], src_f32, cos, ALU.mult, [src_res, cs_res], [res("t1")])
            tt("dve", t2[:, 0:n], ps[:, b, 0:n], sin, ALU.mult, [psr[b], cs_res], [res("t2")])
            tt("pool", out_bf, t1[0:nrows, 0:n], t2[0:nrows, 0:n], ALU.add, [res("t1"), res("t2")], [out_res])

        def _chunk_vars(c):
            return hTc[c % 2], r_hTc[c % 2], c * 512, cst[c % 2], res("cst%d" % (c % 2))

        def xproc_gen(c):
            hc, rh, t0, csb, r_cs = _chunk_vars(c)
            dma("sp", csb, tabs[1, :, :, t0:t0 + 512].rearrange("s p n -> p s n"), [], [r_cs], "cst%d" % (c % 2))
            for q4 in range(4):
                ti = c * 4 + q4
                xx = xt[ti % 2]
                rx = r_xt[ti % 2]
                dma("sp", xx, x[ti * 128:(ti + 1) * 128, :], [], [rx], "xt%d" % (ti % 2))
                act(xn, xx, AF.Square, [rx], [res("xn"), res("st1")], accum=st1[:, 0:1])
                act(st1[:, 1:2], st1[:, 0:1], AF.Sqrt, [res("st1")], [res("st1b")], scale=1.0 / D, bias=EPS)
                recip(st1[:, 2:3], st1[:, 1:2], [res("st1b")], [res("st1c")])
                ts("dve", xn, xx, st1[:, 2:3], None, ALU.mult, None, [rx, res("st1c")], [res("xn")])
                for half in range(2):
                    b = gbank()
                    pb = psb(b)
                    for k8 in range(8):
                        kc = half * 8 + k8
                        tr(pb[:, k8 * 128:(k8 + 1) * 128], xn[:, kc * 128:(kc + 1) * 128], [res("xn"), rc], [psr[b]])
                    for k8 in range(8):
                        kc = half * 8 + k8
                        eng = "dve" if k8 % 2 == 0 else "pool"
                        if eng == "pool":
                            act(hc[:, kc, q4 * 128:(q4 + 1) * 128], pb[:, k8 * 128:(k8 + 1) * 128], AF.Identity, [psr[b], res("AB")], [rh], scale=A1[:, kc:kc + 1], bias=B1[:, kc:kc + 1])
                        else:
                            ts("dve", hc[:, kc, q4 * 128:(q4 + 1) * 128], pb[:, k8 * 128:(k8 + 1) * 128], A1[:, kc:kc + 1], B1[:, kc:kc + 1], ALU.mult, ALU.add, [psr[b], res("AB")], [rh])
                yield
            dma("act", hT_scr[c], hc.rearrange("p a b -> p (a b)"), [rh], [res("hT_scr")], "hTs%d" % (c % 2))

            yield

        def proj_gen(c):
            hc, rh, t0, csb, r_cs = _chunk_vars(c)
            def proj(c0, M, b):
                for kc in range(16):
                    mm(ps[0:M, b, :], Wa[:, kc, c0:c0 + M], hc[:, kc, :], kc == 0, kc == 15, [r_Wa, rh], [psr[b]])

            sb_ = 5
            for j in range(4 if c >= OC0 else 0):
                b = gbank()
                proj(CQ0 + j * 128, 128, b)
                act(sq, ps[:, b, :], AF.Square, [psr[b]], [res("sq")])
                mm(ps[:, sb_, :], ones[:], sq, j == 0, j == 3, [res("sq"), rc], [psr[sb_]])
                act(cqg[:, j, :], ps[:, b, :], AF.Identity, [psr[b], rc], [res("cqg")], scale=gq[:, j:j + 1])
                yield
            if c >= OC0:
                act(rstdb, ps[:, sb_, :], AF.Sqrt, [psr[sb_]], [res("rstdb")], scale=1.0 / 512, bias=EPS)
                recip(rstdb, rstdb, [res("rstdb")], [res("rstdb")])
                dma("pool", rstdq_scr[0:1, t0:t0 + 512], rstdb[0:1, :], [res("rstdb")], [res("rstdq_scr")], "rq")
                dma("act", cqg_scr[:, :, t0:t0 + 512], cqg, [res("cqg")], [res("cqg_scr")], "cqgs")
            bk = [6, 7]
            for j in range(2):
                proj(CKV0 + j * 128, 128, bk[j])
                act(sq, ps[:, bk[j], :], AF.Square, [psr[bk[j]]], [res("sq")])
                mm(ps[:, sb_, :], ones[:], sq, j == 0, j == 1, [res("sq"), rc], [psr[sb_]])
                yield
            act(rstdb, ps[:, sb_, :], AF.Sqrt, [psr[sb_]], [res("rstdb")], scale=1.0 / 256, bias=EPS)
            recip(rstdb, rstdb, [res("rstdb")], [res("rstdb")])
            for j in range(2):
                stt(ckvn[:, j, :], ps[:, bk[j], :], gkv[:, j:j + 1], rstdb, ALU.mult, ALU.mult, [psr[bk[j]], res("rstdb"), rc], [res("ckvn")])
            dma("pool", ckvT_scr[:, :, t0:t0 + 512], ckvn, [res("ckvn")], [res("ckvT_scr")], "ckvs")
            b = gbank()
            proj(KRI0, 112, b)
            cp("act", xq[0:112, :], ps[0:112, b, :], [psr[b]], [res("xq")])
            dma("act", wiT_scr[:, t0:t0 + 512], xq[96:112, :], [res("xq")], [res("wiT_scr")], "wis")
            cp("act", xb[0:112, :], xq[0:112, :], [res("xq")], [res("xb")])
            b2 = gbank()
            mm(ps[0:112, b2, :], rm[0:112, 1, 0:112], xb[0:112, :], True, True, [res("xb"), rc], [psr[b2]])
            tt("pool", t1[0:112, :], xq[0:112, :], csb[0:112, 0, :], ALU.mult, [res("xq"), r_cs], [res("t1")])
            tt("dve", t2[0:112, :], ps[0:112, b2, :], csb[0:112, 1, :], ALU.mult, [psr[b2], r_cs], [res("t2")])
            tt("pool", krio[0:96, :], t1[0:96, :], t2[0:96, :], ALU.add, [res("t1"), res("t2")], [res("krio")])
            dma("pool", kri_scr[:, t0:t0 + 512], krio[0:96, :], [res("krio")], [res("kri_scr")], "kris")
            yield
            for h in range(8):
                b = gbank()
                mm(ps[:, b, :], WukT[:, 0, h, :], ckvn[:, 0, :], True, False, [r_W2, res("ckvn")], [psr[b]])
                mm(ps[:, b, :], WukT[:, 1, h, :], ckvn[:, 1, :], False, False, [r_W2, res("ckvn")], [psr[b]])
                mm(ps[:, b, :], Esel[0:96, :], krio[0:96, :], False, True, [r_W2, res("krio")], [psr[b]])
                cp("act" if h % 2 else "dve", KhTc[:, h, :], ps[:, b, :], [psr[b]], [res("KhTc")])
                yield
            dma("act", KhT_scr[:, :, t0:t0 + 512], KhTc, [res("KhTc")], [res("KhT_scr")], "khs")
            for q4 in range(4):
                for n2 in range(2):
                    b = gbank()
                    for j in range(2):
                        mm(ps[:, b, :], ckvn[:, j, q4 * 128:(q4 + 1) * 128], Wuv2[:, j].rearrange("p h v -> p (h v)")[:, n2 * 512:(n2 + 1) * 512], j == 0, j == 1, [res("ckvn"), r_W2], [psr[b]])
                    cp("act" if n2 else "dve", VHc[:, q4, n2 * 512:(n2 + 1) * 512], ps[:, b, :], [psr[b]], [res("VHc")])
                    yield
            for h in range(8):
                dma("act", VH_scr[h, :, 4 * c:4 * c + 4, :], VHc[:, :, h * 128:(h + 1) * 128], [res("VHc")], [res("VH_scr")], "vhs")
            for j in range(8 if c >= OC0 else 0):
                b = gbank()
                proj(GA0 + j * 128, 128, b)
                g = gao[j % 2]
                act(g, ps[:, b, :], AF.Silu, [psr[b]], [res("gao%d" % (j % 2))])
                dma("act", gaT_scr[:, j, t0:t0 + 512], g, [res("gao%d" % (j % 2))], [res("gaT_scr")], "gas%d" % (j % 2))
                yield

            yield

        def _drain(gen):
            for _ in gen:
                pass

        _drain(xproc_gen(0))
        for c in range(NCH):
            pg = proj_gen(c)
            xg = xproc_gen(c + 1) if c + 1 < NCH else None
            n_ = 0
            for _ in pg:
                n_ += 1
                if xg is not None and n_ % 3 == 0:
                    next(xg, None)
            if xg is not None:
                _drain(xg)
        S.barrier()

        AR.reset()
        kiT = AR.alloc([128, T], BF16)
        Wqi = AR.alloc([128, 4, 1024], BF16)
        cq2 = [AR.alloc([128, 4, 128], BF16) for _ in range(2)]
        cs2 = [AR.alloc([128, 2, 4, 128], F32) for _ in range(2)]
        wtok = [AR.alloc([128, 16], F32) for _ in range(2)]
        rcol = [AR.alloc([128, 2], F32) for _ in range(2)]
        wsc = [AR.alloc([128, 16], F32) for _ in range(2)]
        xq3 = [AR.alloc([128, 512], F32) for _ in range(2)]
        xb3 = [AR.alloc([128, 512], BF16) for _ in range(2)]
        t13 = [AR.alloc([128, 512], F32) for _ in range(2)]
        t23 = [AR.alloc([128, 512], F32) for _ in range(2)]
        qiT = [AR.alloc([128, 8, 128], BF16) for _ in range(2)]
        Dm = [AR.alloc([128, 16, 128], BF16) for _ in range(2)]
        NRL = 3
        rl = [AR.alloc([128, 2, 512], BF16) for _ in range(NRL)]
        isc = [AR.alloc([128, T], F32) for _ in range(2)]
        junk = AR.alloc([128, T], BF16)
        m01 = [AR.alloc([128, T], BF16) for _ in range(2)]
        mT = [AR.alloc([128, NTT, 128], BF16) for _ in range(2)]
        bs = [AR.alloc([128, 8], F32) for _ in range(2)]
        r_kiT, r_Wqi = res("kiT"), res("Wqi")
        dma("sp", kiT[0:64, :], kri_scr[32:96, :], [res("kri_scr")], [r_kiT], "p3Ksp")
        dma("sp", kiT[64:128, :], kri_scr[32:96, :], [res("kri_scr")], [r_kiT], "p3Ksp")
        dma("pool", Wqi, w_uqi.rearrange("(f p) n -> p f n", p=128), [], [r_Wqi], "p3Kpool")
        ISBS = [6, 7]
        isb_cnt = [0]
        rl_cnt = [0]
        pair_cnt = [0]
        LA = 2

        def p3_loads(qb):
            t0 = qb * 128
            s = qb % 2
            r_in = res("p3in%d" % s)
            dma("sp", cq2[s], cqg_scr[:, :, t0:t0 + 128], [res("cqg_scr")], [r_in], "p3in%d" % s)
            for s_ in range(2):
                dma("sp", cs2[s][:, s_, :, :], tabs[2, s_, :, t0:t0 + 128].unsqueeze(1).to_broadcast([128, 4, 128]), [], [r_in], "p3in%d" % s)
            dma("sp", wtok[s], wiT_scr[:, t0:t0 + 128].rearrange("h p -> p h"), [res("wiT_scr")], [r_in], "p3in%d" % s, slow=True)
            dma("sp", rcol[s][:, 0:1], rstdq_scr[0:1, t0:t0 + 128].rearrange("o p -> p o"), [res("rstdq_scr")], [r_in], "p3in%d" % s, slow=True)

        def p3_prologue(qb):
            s = qb % 2
            r_in = res("p3in%d" % s)
            r_q = res("qiT%d" % s)
            for half in range(2):
                hs = half
                b = gbank()
                for jj in range(4):
                    j = half * 4 + jj
                    for f in range(4):
                        mm(ps[:, b, jj * 128:(jj + 1) * 128], Wqi[:, f, j * 128:(j + 1) * 128], cq2[s][:, f, :], f == 0, f == 3, [r_Wqi, r_in], [psr[b]])
                cp("act", xb3[hs], ps[:, b, :], [psr[b]], [res("xb3%d" % hs)])
                cp("act", xq3[hs], ps[:, b, :], [psr[b]], [res("xq3%d" % hs)])
                b2 = gbank()
                mm(ps[:, b2, :], rm[:, 2, :], xb3[hs], True, True, [res("xb3%d" % hs), rc], [psr[b2]])
                tt("pool", t13[hs], xq3[hs], cs2[s][:, 0].rearrange("p a b -> p (a b)"), ALU.mult, [res("xq3%d" % hs), r_in], [res("t13%d" % hs)])
                tt("dve", t23[hs], ps[:, b2, :], cs2[s][:, 1].rearrange("p a b -> p (a b)"), ALU.mult, [psr[b2], r_in], [res("t23%d" % hs)])
                tt("pool", qiT[s][:, half * 4:half * 4 + 4, :].rearrange("p a b -> p (a b)"), t13[hs], t23[hs], ALU.add, [res("t13%d" % hs), res("t23%d" % hs)], [r_q])
            ts("pool", wsc[s], wtok[s], rcol[s][:, 0:1], 1.0 / 32, ALU.mult, ALU.mult, [r_in], [res("wsc%d" % s)])
            for h in range(16):
                ts("pool", Dm[s][:, h, :], ident[:], wsc[s][:, h:h + 1], 1.0, ALU.mult, ALU.mult, [rc, res("wsc%d" % s)], [res("Dm%d" % s)])

        def p3_groups(qb):
            s = qb % 2
            ng = qb // 4 + 1
            items = [(g, hp) for g in range(ng) for hp in range(8)]
            st = {}

            def stageA(g, hp):
                nt = min(4, qb + 1 - 4 * g)
                W = nt * 128
                k0 = g * 512
                pr = pair_cnt[0] % 3
                pair_cnt[0] += 1
                for e in range(2):
                    b = 2 * pr + e
                    mm(ps[:, b, 0:W], qiT[s][64 * e:64 * e + 64, hp, :], kiT[64 * e:64 * e + 64, k0:k0 + W], True, True, [res("qiT%d" % s), r_kiT], [psr[b]])
                ri = rl_cnt[0] % NRL
                rl_cnt[0] += 1
                act(rl[ri][:, :, 0:W], ps[:, 2 * pr:2 * pr + 2, 0:W], AF.Relu, [psr[2 * pr], psr[2 * pr + 1]], [res("rl%d" % ri)])
                st[(g, hp)] = ri

            def stageB(g, hp):
                nt = min(4, qb + 1 - 4 * g)
                W = nt * 128
                k0 = g * 512
                if hp == 0:
                    isb_cnt[0] += 1
                ISB = ISBS[isb_cnt[0] % 2]
                ri = st[(g, hp)]
                for e in range(2):
                    h = 2 * hp + e
                    mm(ps[:, ISB, 0:W], Dm[s][:, h, :], rl[ri][:, e, 0:W], h == 0, h == 15, [res("Dm%d" % s), res("rl%d" % ri)], [psr[ISB]])
                if hp == 7:
                    r_i = res("isc%d" % s)
                    if g == ng - 1:
                        cp("act", isc[s][:, k0:k0 + W], ps[:, ISB, 0:W], [psr[ISB]], [r_i])
                        tt("pool", isc[s][:, k0 + W - 128:k0 + W], isc[s][:, k0 + W - 128:k0 + W], cm[:], ALU.add, [r_i, rc], [r_i])
                    elif g < OC0:
                        act(isc[s][:, k0:k0 + W], ps[:, ISB, 0:W], AF.Identity, [psr[ISB], rc], [r_i], bias=padb[:, 0:1])
                    else:
                        cp("act", isc[s][:, k0:k0 + W], ps[:, ISB, 0:W], [psr[ISB]], [r_i])
            n = len(items)
            for i in range(n + LA):
                if i < n:
                    stageA(*items[i])
                if i - LA >= 0:
                    stageB(*items[i - LA])

        NIT3 = 15

        def p3_bisect(qb):
            s = qb % 2
            Sk = (qb + 1) * 128
            b_ = bs[s]
            r_i = res("isc%d" % s)
            r_bs = res("bs_%d" % s)
            memset("dve", b_[:, 0:1], 0.0, [r_bs])
            for it in range(NIT3):
                Wk = 16.0 / (2 ** it)
                ts("dve", junk[:, 0:Sk], isc[s][:, 0:Sk], b_[:, 0:1], None, ALU.is_ge, ALU.add, [r_i, r_bs], [res("junk"), res("bs2_%d" % s)], accum=b_[:, 2:3])
                ts("dve", b_[:, 3:4], b_[:, 2:3], 255.5, Wk, ALU.is_ge, ALU.mult, [res("bs2_%d" % s)], [res("bs3_%d" % s)])
                ts("dve", b_[:, 0:1], b_[:, 3:4], b_[:, 0:1], -Wk / 2, ALU.add, ALU.add, [r_bs, res("bs3_%d" % s)], [r_bs])
            ts("dve", b_[:, 1:2], b_[:, 0:1], -16.0 / (2 ** NIT3), None, ALU.add, None, [r_bs], [res("bs1_%d" % s)])
            ts("dve", m01[s][:, 0:Sk], isc[s][:, 0:Sk], b_[:, 1:2], None, ALU.is_ge, None, [r_i, res("bs1_%d" % s)], [res("m01_%d" % s)])

        def p3_transposes(qb):
            s = qb % 2
            mt = mT[s]
            r_mt = res("mT%d" % s)
            for g8 in range((qb + 8) // 8):
                n8 = min(8, qb + 1 - 8 * g8)
                b = gbank()
                pb = psb(b)
                for i in range(n8):
                    kt = g8 * 8 + i
                    tr(pb[:, i * 128:(i + 1) * 128], m01[s][:, kt * 128:(kt + 1) * 128], [res("m01_%d" % s), rc], [psr[b]])
                cp("act", mt[:, g8 * 8:g8 * 8 + n8, :].rearrange("p a b -> p (a b)"), pb[:, 0:n8 * 128], [psr[b]], [r_mt])
            c_, j_ = qb // 4, qb % 4
            nk_ = 4 * c_ + 4
            dma("act", maskT_scr[c_, :, 0:nk_, j_ * 128:(j_ + 1) * 128], mt[:, 0:nk_, :], [r_mt], [res("maskT_scr")], "mts%d" % s)

        memset("pool", mT[0][:], 0.0, [res("mT0")])
        memset("pool", mT[1][:], 0.0, [res("mT1")])
        p3_loads(OT0)
        p3_loads(OT0 + 1)
        p3_prologue(OT0)
        for qb in range(OT0, NTT):
            p3_groups(qb)
            if qb + 2 < NTT:
                p3_loads(qb + 2)
            if qb + 1 < NTT:
                p3_prologue(qb + 1)
            p3_bisect(qb)
            if qb >= OT0 + 1:
                p3_transposes(qb - 1)
        p3_transposes(NTT - 1)
        S.barrier()

        AR.reset()
        Wq = AR.alloc([128, 4, 1024], BF16)
        cq4 = AR.alloc([128, 4, 512], BF16)
        rs4 = AR.alloc([128, 512], F32)
        cs4 = AR.alloc([128, 2, 512], F32)
        mTc = AR.alloc([128, NTT, 512], BF16)
        ga4 = AR.alloc([128, 8, 512], BF16)
        xq4 = [AR.alloc([128, 512], F32) for _ in range(2)]
        xb4 = [AR.alloc([128, 512], BF16) for _ in range(2)]
        t14 = [AR.alloc([128, 512], F32) for _ in range(2)]
        t24 = [AR.alloc([128, 512], F32) for _ in range(2)]
        qh = AR.alloc([128, 8, 512], BF16)
        Kh = [AR.alloc([128, T], BF16) for _ in range(2)]
        VH = [AR.alloc([128, NTT, 128], BF16) for _ in range(2)]
        NPB = 3
        PT = [AR.alloc([128, 2, 512], BF16) for _ in range(NPB)]
        PmT = [AR.alloc([128, 2, 512], BF16) for _ in range(NPB)]
        rzb = [AR.alloc([128, 512], F32) for _ in range(2)]
        tmp4 = [AR.alloc([128, 512], F32) for _ in range(2)]
        oA = AR.alloc([128, 8, 512], BF16)
        r_K = res("Kres")
        dma("pool", Wq, w_uq.rearrange("(f p) n -> p f n", p=128), [], [r_K], "p4Kpool")
        ACCS = [[4, 5], [6, 7]]
        gb4 = [0]

        def gbank4():
            gb4[0] += 1
            return gb4[0] % 4
        pcnt = [0]
        hc4 = [0]
        LA4 = 2

        def p4_kv_load(c, h, slot):
            nkt = 4 * c + 4
            r_kv = res("KV%d" % slot)
            dma("sp", Kh[slot][:, 0:nkt * 128], KhT_scr[:, h, 0:nkt * 128], [res("KhT_scr")], [r_kv], "kv%d" % slot)
            dma("sp", VH[slot][:, 0:nkt, :], VH_scr[h, :, 0:nkt, :], [res("VH_scr")], [r_kv], "kv%d" % slot)

        heads_seq = [(c, h) for c in range(OC0, NCH) for h in range(8)]
        p4_kv_load(heads_seq[0][0], heads_seq[0][1], 0)
        for c in range(OC0, NCH):
            t0 = c * 512
            nkt = 4 * c + 4
            r_in = res("p4in")
            r_m = res("p4mask")
            dma("sp", cq4, cqg_scr[:, :, t0:t0 + 512], [res("cqg_scr")], [r_in], "p4in")
            dma("sp", rs4, rstdq_scr[0:1, t0:t0 + 512].partition_broadcast(128), [res("rstdq_scr")], [r_in], "p4in")
            dma("sp", cs4, tabs[0, :, :, t0:t0 + 512].rearrange("s p n -> p s n"), [], [r_in], "p4in")
            dma("sp", ga4, gaT_scr[:, :, t0:t0 + 512], [res("gaT_scr")], [r_in], "p4in")
            dma("sp", mTc[:, 0:nkt, :], maskT_scr[c, :, 0:nkt, :], [res("maskT_scr")], [r_m], "p4mask")
            r_qh = res("qh")
            pend = []
            for h in range(8):
                hs = h % 2
                b = gbank4()
                for f in range(4):
                    mm(ps[:, b, :], Wq[:, f, h * 128:(h + 1) * 128], cq4[:, f, :], f == 0, f == 3, [r_K, r_in], [psr[b]])
                for p_ in pend:
                    p_()
                pend = []
                tt("dve", xq4[hs], ps[:, b, :], rs4, ALU.mult, [psr[b], r_in], [res("xq4%d" % hs)])
                cp("act", xb4[hs], xq4[hs], [res("xq4%d" % hs)], [res("xb4%d" % hs)])

                def part2(h=h, hs=hs):
                    b2 = gbank4()
                    mm(ps[:, b2, :], rm[:, 0, :], xb4[hs], True, True, [res("xb4%d" % hs), rc], [psr[b2]])
                    tt("pool", t14[hs], xq4[hs], cs4[:, 0, :], ALU.mult, [res("xq4%d" % hs), r_in], [res("t14%d" % hs)])
                    tt("dve", t24[hs], ps[:, b2, :], cs4[:, 1, :], ALU.mult, [psr[b2], r_in], [res("t24%d" % hs)])
                    tt("pool", qh[:, h, :], t14[hs], t24[hs], ALU.add, [res("t14%d" % hs), res("t24%d" % hs)], [r_qh])
                pend.append(part2)
            for p_ in pend:
                p_()
            items = [(h, kp) for h in range(8) for kp in range(nkt // 2)]
            st4 = {}
            slot_of = {}

            def stageA(h, kp):
                if kp == 0:
                    slot_of[h] = hc4[0] % 2
                    hc4[0] += 1
                if kp == LA4:
                    idx = heads_seq.index((c, h))
                    if idx + 1 < len(heads_seq):
                        p4_kv_load(heads_seq[idx + 1][0], heads_seq[idx + 1][1], hc4[0] % 2)
                sl = slot_of[h]
                pr = 2 * (pcnt[0] % 2)
                for e in range(2):
                    kt = 2 * kp + e
                    mm(ps[:, pr + e, :], Kh[sl][:, kt * 128:(kt + 1) * 128], qh[:, h, :], True, True, [res("KV%d" % sl), r_qh], [psr[pr + e]])
                pi = pcnt[0] % NPB
                pcnt[0] += 1
                act(PT[pi], ps[:, pr:pr + 2, :], AF.Exp, [psr[pr], psr[pr + 1]], [res("PT%d" % pi)], scale=SCALE)
                tt("pool" if (kp < 2 or pcnt[0] % 4 == 0) else "dve", PmT[pi], PT[pi], mTc[:, 2 * kp:2 * kp + 2, :], ALU.mult, [res("PT%d" % pi), r_m], [res("PmT%d" % pi)])
                st4[(h, kp)] = pi

            def stageB(h, kp):
                pi = st4[(h, kp)]
                sl = slot_of[h]
                ACC = ACCS[sl]
                for e in range(2):
                    kt = 2 * kp + e
                    mm(ps[:, ACC[0], :], VH[sl][:, kt, :], PmT[pi][:, e, :], kt == 0, kt == nkt - 1, [res("KV%d" % sl), res("PmT%d" % pi)], [psr[ACC[0]]])
                for e in range(2):
                    kt = 2 * kp + e
                    mm(ps[:, ACC[1], :], ones[:], PmT[pi][:, e, :], kt == 0, kt == nkt - 1, [rc, res("PmT%d" % pi)], [psr[ACC[1]]])
                if kp == nkt // 2 - 1:
                    recip(rzb[sl], ps[:, ACC[1], :], [psr[ACC[1]]], [res("rzb%d" % sl)])
                    tt("dve", tmp4[sl], ps[:, ACC[0], :], rzb[sl], ALU.mult, [psr[ACC[0]], res("rzb%d" % sl)], [res("tmp4%d" % sl)])
                    tt("pool", oA[:, h, :], tmp4[sl], ga4[:, h, :], ALU.mult, [res("tmp4%d" % sl), r_in], [res("oA")])
            n = len(items)
            for i in range(n + LA4):
                if i < n:
                    stageA(*items[i])
                if i - LA4 >= 0:
                    stageB(*items[i - LA4])
            dma("pool", oT_scr[:, 0:8, t0:t0 + 512], oA, [res("oA")], [res("oT_scr")], "oas")
        S.barrier()

        AR.reset()
        HT = T // 2
        Wb = [AR.alloc([128, 16, 4, 128], BF16) for _ in range(2)]
        hT5 = [AR.alloc([128, 16, 512], BF16) for _ in range(2)]
        cs5 = [AR.alloc([128, 2, 512], F32) for _ in range(2)]
        qT = [AR.alloc([128, HT], BF16) for _ in range(2)]
        kT = [AR.alloc([128, T], BF16) for _ in range(2)]
        vT = [AR.alloc([128, T], BF16) for _ in range(2)]
        gT = [AR.alloc([128, HT], BF16) for _ in range(2)]
        VB = AR.alloc([128, 32, 128], BF16)
        xq5 = [AR.alloc([128, 512], F32) for _ in range(2)]
        xb5 = [AR.alloc([128, 512], BF16) for _ in range(2)]
        t15 = [AR.alloc([128, 512], F32) for _ in range(2)]
        t25 = [AR.alloc([128, 512], F32) for _ in range(2)]
        NP5 = 3
        PT5 = [AR.alloc([128, 512], BF16) for _ in range(NP5)]
        Pm5 = [AR.alloc([128, 512], BF16) for _ in range(NP5)]
        accO = AR.alloc([128, HT], F32)
        accZ = AR.alloc([128, HT], F32)
        oB = AR.alloc([128, HT], BF16)
        COLS = [1904, 2928, 3952, 4976]
        rcnt = [0]
        p5cnt = [0]
        LA5 = 2

        def load_wb(h):
            for i in range(4):
                dma("pool", Wb[h % 2][:, :, i, :], w_in[:, COLS[i] + 128 * h:COLS[i] + 128 * h + 128].rearrange("(kc p) n -> p kc n", p=128), [], [res("Wb%d" % (h % 2))], "wb%d" % (h % 2))

        def proj_gen(h):
            s5 = h % 2
            wbh = Wb[s5]
            r_wb = res("Wb%d" % s5)
            if h + 1 < 8:
                load_wb(h + 1)
            pend5 = []
            for c in range(NCH):
                t0 = c * 512
                hc = hT5[c % 2]
                rh = res("hT5_%d" % (c % 2))
                dma("sp", hc.rearrange("p a b -> p (a b)"), hT_scr[c], [res("hT_scr")], [rh], "h5_%d" % (c % 2))
                csb = cs5[c % 2]
                r_cs = res("cs5_%d" % (c % 2))
                dma("sp", csb, tabs[0, :, :, t0:t0 + 512].rearrange("s p n -> p s n"), [], [r_cs], "c5_%d" % (c % 2))
                for i in range(4):
                    if c < OC0 and i in (0, 3):
                        continue
                    b = gbank()
                    for kc in range(16):
                        mm(ps[:, b, :], wbh[:, kc, i, :], hc[:, kc, :], kc == 0, kc == 15, [r_wb, rh], [psr[b]])
                    for p_ in pend5:
                        p_()
                    pend5 = []
                    if i < 2:
                        if i == 0:
                            dst = qT[s5][:, t0 - HT:t0 - HT + 512]
                            r_dst = res("qT%d" % s5)
                        else:
                            dst = kT[s5][:, t0:t0 + 512]
                            r_dst = res("kT%d" % s5)
                        ri = rcnt[0] % 2
                        rcnt[0] += 1
                        cp("act", xb5[ri], ps[:, b, :], [psr[b]], [res("xb5%d" % ri)])
                        cp("act", xq5[ri], ps[:, b, :], [psr[b]], [res("xq5%d" % ri)])

                        def part2(ri=ri, dst=dst, csb=csb, r_cs=r_cs, r_dst=r_dst):
                            b2 = gbank()
                            mm(ps[:, b2, :], rm[:, 0, :], xb5[ri], True, True, [res("xb5%d" % ri), rc], [psr[b2]])
                            tt("pool", t15[ri], xq5[ri], csb[:, 0, :], ALU.mult, [res("xq5%d" % ri), r_cs], [res("t15%d" % ri)])
                            tt("dve", t25[ri], ps[:, b2, :], csb[:, 1, :], ALU.mult, [psr[b2], r_cs], [res("t25%d" % ri)])
                            tt("pool", dst, t15[ri], t25[ri], ALU.add, [res("t15%d" % ri), res("t25%d" % ri)], [r_dst])
                        pend5.append(part2)
                    elif i == 2:
                        cp("dve", vT[s5][:, t0:t0 + 512], ps[:, b, :], [psr[b]], [res("vT%d" % s5)])
                    else:
                        act(gT[s5][:, t0 - HT:t0 - HT + 512], ps[:, b, :], AF.Silu, [psr[b]], [res("gT%d" % s5)])
                    yield
            for p_ in pend5:
                p_()
            yield

        def attn_gen(h):
            s5 = h % 2
            r_q, r_k, r_v, r_g = res("qT%d" % s5), res("kT%d" % s5), res("vT%d" % s5), res("gT%d" % s5)
            first = True
            for d in (1, 4, 16):
                nblk = T // (128 * d)
                for g8 in range(4):
                    b = gbank()
                    pb = psb(b)
                    for i in range(8):
                        ti = g8 * 8 + i
                        r_, bl = ti // nblk, ti % nblk
                        st_ = r_ + d * 128 * bl
                        src = vT[s5][:, st_:st_ + 127 * d + 1:d]
                        tr(pb[:, i * 128:(i + 1) * 128], src, [r_v, rc], [psr[b]])
                    cp("act" if g8 % 2 else "dve", VB[:, g8 * 8:g8 * 8 + 8, :].rearrange("p a b -> p (a b)"), pb[:, 0:1024], [psr[b]], [res("VB")])
                    yield
                qtiles = [(r_, bl) for r_ in range(d) for bl in range(nblk // 2, nblk)]
                pairs = [qtiles[i:i + 2] for i in range(0, 16, 2)]
                stp = {}

                def stageA(pi_, pair):
                    b = gbank()
                    slots = []
                    for qi_, (r_, bl) in enumerate(pair):
                        st_q = r_ + d * 128 * bl - HT
                        qap = qT[s5][:, st_q:st_q + 127 * d + 1:d]
                        for kk in range(2):
                            kb_ = bl - 1 + kk
                            sl = qi_ * 2 + kk
                            st_k = r_ + d * 128 * kb_
                            kap = kT[s5][:, st_k:st_k + 127 * d + 1:d]
                            mm(ps[:, b, sl * 128:(sl + 1) * 128], kap, qap, True, True, [r_k, r_q], [psr[b]])
                            slots.append((sl, qi_, r_ * nblk + kb_))
                    pp = p5cnt[0] % NP5
                    p5cnt[0] += 1
                    act(PT5[pp], ps[:, b, :], AF.Exp, [psr[b]], [res("PT5%d" % pp)], scale=SCALE)
                    npad = sum(1 for (r_, bl) in pair if bl == nblk // 2)
                    bsel = 0 if npad == 0 else (2 if npad == 2 else 1)
                    assert npad != 1 or pair[0][1] == nblk // 2
                    tt("pool" if pp % 2 else "dve", Pm5[pp], PT5[pp], band[:, bsel, :], ALU.mult, [res("PT5%d" % pp), rc], [res("Pm5%d" % pp)])
                    stp[pi_] = (pp, slots)

                def stageB(pi_, pair, first=first):
                    pp, slots = stp[pi_]
                    b3 = gbank()
                    for qi_, (r_, bl) in enumerate(pair):
                        mine = [s_ for s_ in slots if s_[1] == qi_]
                        for n_, (sl, _, vt_) in enumerate(mine):
                            mm(ps[:, b3, qi_ * 128:(qi_ + 1) * 128], VB[:, vt_, :], Pm5[pp][:, sl * 128:(sl + 1) * 128], n_ == 0, n_ == len(mine) - 1, [res("VB"), res("Pm5%d" % pp)], [psr[b3]])
                        for n_, (sl, _, vt_) in enumerate(mine):
                            mm(ps[:, b3, 256 + qi_ * 128:256 + (qi_ + 1) * 128], ones[:], Pm5[pp][:, sl * 128:(sl + 1) * 128], n_ == 0, n_ == len(mine) - 1, [rc, res("Pm5%d" % pp)], [psr[b3]])
                    for qi_, (r_, bl) in enumerate(pair):
                        st_q = r_ + d * 128 * bl - HT
                        sl_o = slice(st_q, st_q + 127 * d + 1, d)
                        if first:
                            cp("act", accO[:, sl_o], ps[:, b3, qi_ * 128:(qi_ + 1) * 128], [psr[b3]], [res("accO")])
                            cp("act", accZ[:, sl_o], ps[:, b3, 256 + qi_ * 128:256 + (qi_ + 1) * 128], [psr[b3]], [res("accZ")])
                        else:
                            tt("dve", accO[:, sl_o], accO[:, sl_o], ps[:, b3, qi_ * 128:(qi_ + 1) * 128], ALU.add, [psr[b3], res("accO")], [res("accO")])
                            tt("dve", accZ[:, sl_o], accZ[:, sl_o], ps[:, b3, 256 + qi_ * 128:256 + (qi_ + 1) * 128], ALU.add, [psr[b3], res("accZ")], [res("accZ")])
                n = len(pairs)
                for i in range(n + LA5):
                    if i < n:
                        stageA(i, pairs[i])
                    if i - LA5 >= 0:
                        stageB(i - LA5, pairs[i - LA5])
                    yield
                first = False
            for c4 in range(4):
                sl_ = slice(c4 * 512, (c4 + 1) * 512)
                recip(accZ[:, sl_], accZ[:, sl_], [res("accZ")], [res("accZ")])
                tt("dve", accO[:, sl_], accO[:, sl_], accZ[:, sl_], ALU.mult, [res("accO"), res("accZ")], [res("accO")])
                tt("pool", oB[:, sl_], accO[:, sl_], gT[s5][:, sl_], ALU.mult, [res("accO"), r_g], [res("oB")])
                yield
            dma("pool", oT_scr[:, 8 + h, HT:T], oB, [res("oB")], [res("oT_scr")], "obs")
            yield

        def drain(gen):
            for _ in gen:
                pass

        load_wb(0)
        drain(proj_gen(0))
        for h in range(8):
            ag = attn_gen(h)
            if h + 1 < 8:
                pg = proj_gen(h + 1)
                for _ in pg:
                    next(ag, None)
                    next(ag, None)
            drain(ag)
        S.barrier()

        AR.reset()
        Wo = AR.alloc([128, 16, D], BF16)
        oT6 = [AR.alloc([128, 16, 128], BF16) for _ in range(2)]
        x6 = [AR.alloc([128, D], F32) for _ in range(2)]
        y6 = [AR.alloc([128, D], F32) for _ in range(2)]
        jk6 = AR.alloc([128, 512], BF16)
        s6 = AR.alloc([128, 16], F32)
        r_Wo = res("Wo")
        for i in range(4):
            dma("pool", Wo[:, :, i * 512:(i + 1) * 512], w_out[:, i * 512:(i + 1) * 512].rearrange("(kc p) n -> p kc n", p=128), [], [r_Wo], "wo")
        fin = []
        for ti in range(OT0, NTT):
            t0 = ti * 128
            s = ti % 2
            YB = [4, 5, 6, 7] if s else [0, 1, 2, 3]
            r_in = res("p6in%d" % s)
            dma("sp", oT6[s], oT_scr[:, :, t0:t0 + 128], [res("oT_scr")], [r_in], "p6a%d" % s)
            dma("sp", x6[s], x[t0:t0 + 128, :], [], [res("x6_%d" % s)], "p6b%d" % s)
            for n in range(4):
                for kc in range(16):
                    mm(ps[:, YB[n], :], oT6[s][:, kc, :], Wo[:, kc, n * 512:(n + 1) * 512], kc == 0, kc == 15, [r_in, r_Wo], [psr[YB[n]]])
                act(jk6, ps[:, YB[n], :], AF.Square, [psr[YB[n]]], [res("jk6"), res("s6_%d" % n)], accum=s6[:, n:n + 1])
            tt("dve", s6[:, 4:5], s6[:, 0:1], s6[:, 1:2], ALU.add, [res("s6_0"), res("s6_1")], [res("s6a")])
            tt("dve", s6[:, 5:6], s6[:, 2:3], s6[:, 3:4], ALU.add, [res("s6_2"), res("s6_3")], [res("s6b")])
            tt("dve", s6[:, 6:7], s6[:, 4:5], s6[:, 5:6], ALU.add, [res("s6a"), res("s6b")], [res("s6c")])
            act(s6[:, 7:8], s6[:, 6:7], AF.Sqrt, [res("s6c")], [res("s6d")], scale=1.0 / D, bias=EPS)
            recip(s6[:, 8:9], s6[:, 7:8], [res("s6d")], [res("s6e")])
            yy = y6[s]
            r_yy = res("y6_%d" % s)
            for n in range(4):
                sl_ = slice(n * 512, (n + 1) * 512)
                stt(yy[:, sl_], ps[:, YB[n], :], s6[:, 8:9], Gbc[:, sl_], ALU.mult, ALU.mult, [psr[YB[n]], res("s6e"), res("Gbc")], [r_yy])
                tt("pool", yy[:, sl_], yy[:, sl_], x6[s][:, sl_], ALU.add, [r_yy, res("x6_%d" % s)], [r_yy])
            fin.append(dma("pool", out[t0 - OT0 * 128:t0 - OT0 * 128 + 128, :], yy, [r_yy], [res("out")], "outs%d" % s))
        S.emit(nc, final_waits=fin[-2:])
    return nc


def _host_inputs(inp):
    ident = np.eye(128, dtype=np.float32)
    f = lambda a: np.ascontiguousarray(np.asarray(a, dtype=np.float32))
    shared = {
        "w_ada": f(inp["w_ada"][0]),
        "bada_col": f(np.asarray(inp["b_ada"][0])[:4096].reshape(32, 128).T),
        "bada_g": f(np.asarray(inp["b_ada"][0])[4096:].reshape(1, D)),
        "gpre_col": f(np.asarray(inp["g_pre"][0]).reshape(16, 128).T),
        "gpost": f(np.asarray(inp["g_post"][0]).reshape(1, D)),
        "gq_col": f(np.asarray(inp["g_q"][0]).reshape(4, 128).T),
        "gkv_col": f(np.asarray(inp["g_kv"][0]).reshape(2, 128).T),
        "w_in": f(inp["w_in"][0]),
        "w_uq": f(inp["w_uq"][0]),
        "w_uqi": f(inp["w_uq_idx"][0]),
        "w_uk": f(inp["w_uk"][0]),
        "w_uv": f(inp["w_uv"][0]),
        "w_out": f(inp["w_out"][0]),
        "ident": ident,
    }
    per_g = []
    for g in range(2):
        tabs, rmats = _rope_tables(g)
        band, cm = _masks(g)
        padb = np.full((128, 512), 0.0 if g == 1 else NEG, np.float32)
        per_g.append({"tabs": tabs, "rmats": rmats, "band": f(band), "cm": f(cm), "padb": padb})
    maps = []
    xx = np.asarray(inp["x"], dtype=np.float32)
    cc = np.asarray(inp["c"], dtype=np.float32)
    for b in range(4):
        for g in range(2):
            m = dict(shared)
            m.update(per_g[g])
            if g == 1:
                m["x"] = np.ascontiguousarray(xx[b])
            else:
                xw = np.zeros((T, D), np.float32)
                xw[T // 2:] = xx[b, :T // 2]
                m["x"] = xw
            m["scol"] = np.ascontiguousarray(cc[b].reshape(16, 128).T)
            maps.append(m)
    return maps


_NC = None


def kernel(**inputs):
    global _NC
    if _NC is None:
        _NC = build()
    maps = _host_inputs(inputs)
    res_ = run_bass_kernel_spmd(_NC, maps, core_ids=list(range(8)))
    out = np.empty((4, T, D), np.float32)
    for b in range(4):
        for g in range(2):
            out[b, g * (T // 2):(g + 1) * (T // 2)] = np.asarray(res_.results[2 * b + g]["out"], dtype=np.float32)
    return out
```

```python
import contextlib
import numpy as np
import concourse.bass as bass
import concourse.mybir as mybir
from concourse.bass_utils import run_bass_kernel_spmd

F32 = mybir.dt.float32
BF16 = mybir.dt.bfloat16
AF = mybir.ActivationFunctionType
ALU = mybir.AluOpType
AX = mybir.AxisListType

T = 4096
D = 2048
KC = 16
NCH = 8
NTT = 32
EPS = 1e-6
THETA = 500000.0
NIT = 20
OT0 = 16
OC0 = 4
NEG = -1.0e30
SCALE = 128.0 ** -0.5


class Res:
    __slots__ = ("name", "writers", "readers")

    def __init__(self, name):
        self.name = name
        self.writers = {}
        self.readers = []


class Op:
    __slots__ = ("eng", "fn", "deps", "users", "sem", "val", "is_dma", "slot")

    def __init__(self, eng, fn):
        self.eng = eng
        self.fn = fn
        self.deps = []
        self.users = 0
        self.sem = None
        self.val = 0
        self.is_dma = False
        self.slot = None


class Sched:
    ENGS = ("pe", "act", "dve", "pool", "sp")

    def __init__(self):
        self.ops = {e: [] for e in self.ENGS}
        self.slots = {}
        self.last = {e: None for e in self.ENGS}
        self.pending_dma = []
        self.slotinfo = {}

    def add(self, eng, fn, reads=(), writes=(), dma_slot=None):
        op = Op(eng, fn)
        raw = []
        oth = []
        for r in reads:
            raw.extend(r.writers.values())
        for w in writes:
            if eng != "pe":
                raw.extend(w.writers.values())
            else:
                oth.extend(w.writers.values())
            oth.extend(w.readers)
        seen = set()
        for d in raw:
            if id(d) in seen or d is op:
                continue
            if d.eng == eng and not d.is_dma and eng == "pe":
                continue
            seen.add(id(d))
            op.deps.append(d)
        for d in oth:
            if id(d) in seen or d is op:
                continue
            if d.eng == eng and not d.is_dma and eng != "pool" and dma_slot is None:
                continue
            seen.add(id(d))
            op.deps.append(d)
        ups = []
        seen2 = set()
        for d in op.deps:
            if d.is_dma:
                info = self.slotinfo[d.slot]
                d = info["last"]
                info["waited"] = d
            if id(d) not in seen2:
                seen2.add(id(d))
                ups.append(d)
        op.deps = ups
        if dma_slot is not None:
            op.is_dma = True
            op.slot = dma_slot
            info = self.slotinfo.setdefault(dma_slot, {"last": None, "waited": None})
            w_ = info["waited"]
            if w_ is not None and id(w_) not in seen2:
                op.deps.append(w_)
            info["last"] = op
            self.pending_dma.append(op)
        for d in op.deps:
            d.users += 1
        wkey = dma_slot if dma_slot is not None else eng
        for w in writes:
            w.writers[wkey] = op
            w.readers = []
        for r in reads:
            r.readers.append(op)
        self.ops[eng].append(op)
        self.last[eng] = op
        return op

    def barrier(self):
        lasts = [self.last[e] for e in self.ENGS if self.last[e] is not None]
        dmas = []
        for d in self.pending_dma:
            info = self.slotinfo[d.slot]
            d2 = info["last"]
            info["waited"] = d2
            if all(d2 is not x for x in dmas):
                dmas.append(d2)
        self.pending_dma = []
        for e in self.ENGS:
            op = Op(e, lambda eng: eng.nop())
            for d in lasts + dmas:
                if d.eng == e and not d.is_dma and e != "pool":
                    continue
                op.deps.append(d)
                d.users += 1
            self.ops[e].append(op)
            self.last[e] = op

    def emit(self, nc, final_waits=()):
        with contextlib.ExitStack() as st:
            esem = {e: st.enter_context(nc.semaphore("s_" + e)) for e in self.ENGS}
            for e in self.ENGS:
                for op in self.ops[e]:
                    if op.is_dma and op.slot not in self.slots:
                        self.slots[op.slot] = [st.enter_context(nc.semaphore("d_%d" % len(self.slots))), 0]
            for e in self.ENGS:
                cnt = 0
                for op in self.ops[e]:
                    if op.is_dma:
                        s = self.slots[op.slot]
                        s[1] += 16
                        op.sem = s[0]
                        op.val = s[1]
                    elif op.users > 0:
                        cnt += 1
                        op.sem = esem[e]
                        op.val = cnt
            block = st.enter_context(nc.Block())

            def run(e, engine):
                seen = {}
                for op in self.ops[e]:
                    for d in op.deps:
                        k = id(d.sem)
                        if seen.get(k, 0) >= d.val:
                            continue
                        engine.wait_ge(d.sem, d.val)
                        seen[k] = d.val
                    inst = op.fn(engine)
                    if op.is_dma:
                        inst.then_inc(op.sem, 16)
                    elif op.users > 0:
                        inst.then_inc(op.sem, 1)
                if e == "sp":
                    for d in final_waits:
                        k = id(d.sem)
                        if seen.get(k, 0) >= d.val:
                            continue
                        engine.wait_ge(d.sem, d.val)
                        seen[k] = d.val

            @block.tensor
            def _(eng):
                run("pe", eng)

            @block.scalar
            def _(eng):
                run("act", eng)

            @block.vector
            def _(eng):
                run("dve", eng)

            @block.gpsimd
            def _(eng):
                run("pool", eng)

            @block.sync
            def _(eng):
                run("sp", eng)


def _rope_tables(g):
    pos = np.maximum(np.arange(T, dtype=np.float32) + 2048.0 * (g - 1), 0.0).astype(np.float32)
    inv32 = (THETA ** (-np.arange(0, 32, 2, dtype=np.float32) / 32)).astype(np.float32)
    inv16 = (THETA ** (-np.arange(0, 16, 2, dtype=np.float32) / 16)).astype(np.float32)
    a32 = (pos[None, :] * inv32[:, None]).astype(np.float32)
    a16 = (pos[None, :] * inv16[:, None]).astype(np.float32)
    tabs = np.zeros((3, 2, 128, T), np.float32)
    tabs[:, 0] = 1.0
    tabs[0, 0, 0:16] = np.cos(a32); tabs[0, 0, 16:32] = np.cos(a32)
    tabs[0, 1, 0:16] = np.sin(a32); tabs[0, 1, 16:32] = np.sin(a32)
    tabs[1, 0, 0:32] = tabs[0, 0, 0:32]; tabs[1, 1, 0:32] = tabs[0, 1, 0:32]
    tabs[1, 0, 32:40] = np.cos(a16); tabs[1, 0, 40:48] = np.cos(a16)
    tabs[1, 1, 32:40] = np.sin(a16); tabs[1, 1, 40:48] = np.sin(a16)
    for b in (0, 64):
        tabs[2, 0, b:b + 8] = np.cos(a16); tabs[2, 0, b + 8:b + 16] = np.cos(a16)
        tabs[2, 1, b:b + 8] = np.sin(a16); tabs[2, 1, b + 8:b + 16] = np.sin(a16)
    rm = np.zeros((3, 128, 128), np.float32)

    def blk(R, base, half):
        for j in range(half):
            R[base + j + half, base + j] = -1.0
            R[base + j, base + j + half] = 1.0
    blk(rm[0], 0, 16)
    blk(rm[1], 0, 16); blk(rm[1], 32, 8)
    blk(rm[2], 0, 8); blk(rm[2], 64, 8)
    return tabs, rm


def _masks(g):
    k = np.arange(128)[:, None]
    q = np.arange(128)[None, :]
    prev = (k >= q).astype(np.float32)
    cur = (k <= q).astype(np.float32)
    pp = prev * float(g)
    band = np.stack([np.concatenate([prev, cur, prev, cur], axis=1),
                     np.concatenate([pp, cur, prev, cur], axis=1),
                     np.concatenate([pp, cur, pp, cur], axis=1)], axis=0)
    cm = np.where(np.arange(128)[None, :] <= np.arange(128)[:, None], 0.0, NEG).astype(np.float32)
    return band, cm


def build(debug=False):
    nc = bass.Bass("TRN2", target_bir_lowering=False)
    S = Sched()
    okind = "ExternalOutput" if debug else "Internal"

    def din(name, shape, dt=F32):
        return nc.dram_tensor(name, shape, dt, kind="ExternalInput").ap()

    def dscr(name, shape, dt):
        return nc.dram_tensor(name, shape, dt, kind=okind).ap()

    x = din("x", [T, D])
    scol = din("scol", [128, 16])
    w_ada = din("w_ada", [D, 3 * D])
    bada_col = din("bada_col", [128, 32])
    bada_g = din("bada_g", [1, D])
    gpre_col = din("gpre_col", [128, 16])
    gpost = din("gpost", [1, D])
    gq_col = din("gq_col", [128, 4])
    gkv_col = din("gkv_col", [128, 2])
    w_in = din("w_in", [D, 6000])
    w_uq = din("w_uq", [512, 1024])
    w_uqi = din("w_uqi", [512, 1024])
    w_uk = din("w_uk", [8, 96, 256])
    w_uv = din("w_uv", [8, 256, 128])
    w_out = din("w_out", [D, D])
    tabs = din("tabs", [3, 2, 128, T])
    rmats = din("rmats", [3, 128, 128])
    ident_d = din("ident", [128, 128])
    band_d = din("band", [3, 128, 512])
    padb_d = din("padb", [128, 512])
    cm_d = din("cm", [128, 128])
    out = nc.dram_tensor("out", [T // 2, D], F32, kind="ExternalOutput").ap()

    hT_scr = dscr("hT_scr", [128, KC, T], BF16)
    cqg_scr = dscr("cqg_scr", [128, 4, T], BF16)
    rstdq_scr = dscr("rstdq_scr", [1, T], F32)
    ckvT_scr = dscr("ckvT_scr", [128, 2, T], BF16)
    KhT_scr = dscr("KhT_scr", [128, 8, T], BF16)
    VH_scr = dscr("VH_scr", [8, 128, NTT, 128], BF16)
    kri_scr = dscr("kri_scr", [96, T], BF16)
    wiT_scr = dscr("wiT_scr", [16, T], F32)
    gaT_scr = dscr("gaT_scr", [128, 8, T], BF16)
    maskT_scr = dscr("maskT_scr", [NCH, 128, NTT, 512], BF16)
    oT_scr = dscr("oT_scr", [128, 16, T], BF16)

    R = {}

    def res(name):
        if name not in R:
            R[name] = Res(name)
        return R[name]

    with contextlib.ExitStack() as st:
        ps = st.enter_context(nc.psum_tensor("ps", [128, 8, 512], F32))
        psr = [res("psb%d" % i) for i in range(8)]
        gb = [0]

        def gbank(n=5):
            b = gb[0] % n
            gb[0] += 1
            return b

        def psb(b):
            return ps[:, b, :].bitcast(BF16)

        def sbp(name, shape, dt):
            return st.enter_context(nc.sbuf_tensor("sb_" + name, shape, dt))

        ident = sbp("identb", [128, 128], BF16)
        identf = sbp("identf", [128, 128], F32)
        ones = sbp("onesb", [128, 128], BF16)
        rm = sbp("rm", [128, 3, 128], BF16)
        band = sbp("bandb", [128, 3, 512], BF16)
        padb = sbp("padb", [128, 512], F32)
        cm = sbp("cm", [128, 128], F32)
        A1 = sbp("A1", [128, 16], F32)
        B1 = sbp("B1", [128, 16], F32)
        Gbc = sbp("Gbc", [128, D], F32)
        gq = sbp("gq", [128, 4], F32)
        gkv = sbp("gkv", [128, 2], F32)
        arena = sbp("arena", [128, 88576], BF16)

        class Arena:
            def __init__(self):
                self.off = 0

            def reset(self):
                self.off = 0

            def alloc(self, shape, dt):
                n = int(np.prod(shape[1:]))
                nb = n * (4 if dt == F32 else 2)
                nb = (nb + 63) // 64 * 64
                o = self.off
                self.off += nb // 2
                assert self.off <= 88576, self.off
                v = arena[0:shape[0], o:o + nb // 2]
                if dt == F32:
                    v = v.bitcast(F32)[:, 0:n]
                else:
                    v = v[:, 0:n]
                if len(shape) == 3:
                    v = v.rearrange("p (a b) -> p a b", a=shape[1])
                elif len(shape) == 4:
                    v = v.rearrange("p (a b c) -> p a b c", a=shape[1], b=shape[2])
                return v

        AR = Arena()

        def dma(eng, o, i, reads, writes, slot, slow=False):
            if slow:
                return S.add(eng, lambda e: e.dma_start(out=o, in_=i, allow_slow_non_contiguous=True), reads, writes, dma_slot=slot)
            return S.add(eng, lambda e: e.dma_start(out=o, in_=i), reads, writes, dma_slot=slot)

        def mm(o, l, r, start, stop, reads, writes):
            return S.add("pe", lambda e: e.matmul(o, lhsT=l, rhs=r, start=start, stop=stop), reads, writes)

        def tr(o, i, reads, writes, idt=None):
            idt = ident[:] if idt is None else idt
            return S.add("pe", lambda e: e.transpose(o, i, idt), reads, writes)

        def act(o, i, func, reads, writes, scale=1.0, bias=0.0, accum=None):
            if accum is None:
                return S.add("act", lambda e: e.activation(out=o, in_=i, func=func, scale=scale, bias=bias), reads, writes)
            return S.add("act", lambda e: e.activation(out=o, in_=i, func=func, scale=scale, bias=bias, accum_out=accum), reads, writes)

        def tt(eng, o, a, b, op, reads, writes):
            return S.add(eng, lambda e: e.tensor_tensor(out=o, in0=a, in1=b, op=op), reads, writes)

        def ts(eng, o, a, s1, s2, op0, op1, reads, writes, accum=None):
            if accum is None:
                if s2 is None:
                    return S.add(eng, lambda e: e.tensor_scalar(out=o, in0=a, scalar1=s1, scalar2=None, op0=op0), reads, writes)
                return S.add(eng, lambda e: e.tensor_scalar(out=o, in0=a, scalar1=s1, scalar2=s2, op0=op0, op1=op1), reads, writes)
            return S.add(eng, lambda e: e.tensor_scalar(out=o, in0=a, scalar1=s1, scalar2=s2, op0=op0, op1=op1, accum_out=accum), reads, writes)

        def stt(o, a, s, b, op0, op1, reads, writes):
            return S.add("dve", lambda e: e.scalar_tensor_tensor(out=o, in0=a, scalar=s, in1=b, op0=op0, op1=op1), reads, writes)

        def recip(o, i, reads, writes):
            return S.add("dve", lambda e: e.reciprocal(out=o, in_=i), reads, writes)

        def cp(eng, o, i, reads, writes):
            if eng == "act":
                return S.add("act", lambda e: e.copy(out=o, in_=i), reads, writes)
            return S.add(eng, lambda e: e.tensor_copy(out=o, in_=i), reads, writes)

        def memset(eng, o, v, writes):
            return S.add(eng, lambda e: e.memset(o, v), (), writes)

        rc = res("consts")
        dma("pool", ident[:], ident_d, [], [rc], "cpool")
        dma("sp", identf[:], ident_d, [], [rc], "csp")
        dma("pool", rm[:], rmats.rearrange("t p m -> p t m"), [], [rc], "cpool")
        dma("pool", band[:], band_d.rearrange("t p n -> p t n"), [], [rc], "cpool")
        dma("sp", padb[:], padb_d, [], [rc], "csp")
        dma("sp", cm[:], cm_d, [], [rc], "csp")
        dma("sp", gq[:], gq_col, [], [rc], "csp")
        dma("sp", gkv[:], gkv_col, [], [rc], "csp")
        memset("dve", ones[:], 1.0, [rc])

        AR.reset()
        sc = AR.alloc([128, 16], F32)
        scb = AR.alloc([128, 16], BF16)
        screp = AR.alloc([128, 16, 128], BF16)
        bcol = AR.alloc([128, 32], F32)
        gpc = AR.alloc([128, 16], F32)
        modc = AR.alloc([128, 32], F32)
        bg = AR.alloc([128, D], F32)
        gp = AR.alloc([128, D], F32)
        wblk = [AR.alloc([128, 16, 512], BF16) for _ in range(2)]
        r_sc, r_mod, r_bg = res("sc"), res("modc"), res("bg")
        r_wblk = [res("wblk0"), res("wblk1")]
        dma("sp", sc, scol, [], [r_sc], "p0")
        dma("sp", bcol, bada_col, [], [r_sc], "p0")
        dma("sp", gpc, gpre_col, [], [r_sc], "p0")
        dma("sp", bg, bada_g.partition_broadcast(128), [], [r_bg], "p0")
        dma("sp", gp, gpost.partition_broadcast(128), [], [r_bg], "p0")
        act(sc, sc, AF.Silu, [r_sc], [r_sc])
        cp("dve", scb, sc, [r_sc], [r_sc])
        for kc in range(16):
            ts("dve", screp[:, kc, :], ones[:], sc[:, kc:kc + 1], None, ALU.mult, None, [r_sc, rc], [res("screp")])
        mb = 7
        for blk in range(12):
            wb = wblk[blk % 2]
            rw = r_wblk[blk % 2]
            dma("pool", wb, w_ada[:, blk * 512:(blk + 1) * 512].rearrange("(kc p) n -> p kc n", p=128), [], [rw], "wblk%d" % (blk % 2))
            if blk < 8:
                for jj in range(4):
                    j = blk * 4 + jj
                    for kc in range(16):
                        mm(ps[:, mb, j:j + 1], wb[:, kc, jj * 128:(jj + 1) * 128], scb[:, kc:kc + 1], kc == 0, kc == 15, [rw, r_sc], [psr[mb]])
            else:
                b = gbank()
                for kc in range(16):
                    mm(ps[:, b, :], screp[:, kc, :], wb[:, kc, :], kc == 0, kc == 15, [rw, res("screp")], [psr[b]])
                c0 = (blk - 8) * 512
                tt("dve", Gbc[:, c0:c0 + 512], ps[:, b, :], bg[:, c0:c0 + 512], ALU.add, [psr[b], r_bg], [res("Gbc")])
                tt("pool", Gbc[:, c0:c0 + 512], Gbc[:, c0:c0 + 512], gp[:, c0:c0 + 512], ALU.mult, [res("Gbc"), r_bg], [res("Gbc")])
        tt("dve", modc, ps[:, mb, 0:32], bcol, ALU.add, [psr[mb], r_sc], [r_mod])
        cp("dve", B1[:], modc[:, 0:16], [r_mod], [res("AB")])
        ts("dve", modc[:, 16:32], modc[:, 16:32], 1.0, None, ALU.add, None, [r_mod], [r_mod])
        tt("dve", A1[:], modc[:, 16:32], gpc, ALU.mult, [r_mod, r_sc], [res("AB")])
        S.barrier()

        AR.reset()
        Wa = AR.alloc([128, 16, 1904], BF16)
        xt = [AR.alloc([128, D], F32) for _ in range(2)]
        xn = AR.alloc([128, D], BF16)
        hTc = [AR.alloc([128, 16, 512], BF16) for _ in range(2)]
        st1 = AR.alloc([128, 8], F32)
        sq = AR.alloc([128, 512], BF16)
        rstdb = AR.alloc([128, 512], F32)
        cqg = AR.alloc([128, 4, 512], BF16)
        ckvn = AR.alloc([128, 2, 512], BF16)
        WukT = AR.alloc([128, 2, 8, 128], BF16)
        Wukn = AR.alloc([128, 8, 256], BF16)
        Wuv2 = AR.alloc([128, 2, 8, 128], BF16)
        Esel = AR.alloc([128, 128], BF16)
        KhTc = AR.alloc([128, 8, 512], BF16)
        VHc = AR.alloc([128, 4, 1024], BF16)
        xq = AR.alloc([128, 512], F32)
        xb = AR.alloc([128, 512], BF16)
        t1 = AR.alloc([128, 512], F32)
        t2 = AR.alloc([128, 512], F32)
        krio = AR.alloc([128, 512], BF16)
        gao = [AR.alloc([128, 512], BF16) for _ in range(2)]
        cst = [AR.alloc([128, 2, 512], F32) for _ in range(2)]
        r_Wa = res("Wa")
        r_xt = [res("xt0"), res("xt1")]
        r_hTc = [res("hTc0"), res("hTc1")]
        for i in range(4):
            dma("pool", Wa[:, :, i * 476:(i + 1) * 476], w_in[:, i * 476:(i + 1) * 476].rearrange("(kc p) n -> p kc n", p=128), [], [r_Wa], "Wa")
        CQ0, CKV0, KRI0, GA0 = 0, 512, 768, 880
        r_W2 = res("W2")
        dma("pool", Wukn[0:96, :, :], w_uk.rearrange("h n r -> n h r"), [], [res("Wukn")], "W2")
        for j in range(2):
            dma("pool", Wuv2[:, j], w_uv[:, j * 128:(j + 1) * 128, :].rearrange("h p v -> p h v"), [], [r_W2], "W2")
        memset("pool", WukT, 0.0, [r_W2])
        memset("pool", Esel, 0.0, [r_W2])
        cp("pool", Esel[0:32, 0:32], ident[0:32, 0:32], [rc], [r_W2])
        for j in range(2):
            b = gbank()
            pb = psb(b)
            for h in range(8):
                tr(pb[:, h * 96:(h + 1) * 96], Wukn[0:96, h, j * 128:(j + 1) * 128], [res("Wukn"), rc], [psr[b]], idt=ident[0:96, 0:96])
            cp("dve", WukT[:, j, :, 32:128], pb[:, 0:768].rearrange("p (h n) -> p h n", h=8), [psr[b]], [r_W2])

        def rope(src_f32, src_res, typ, cos, sin, cs_res, out_bf, out_res, nrows=128):
            n = src_f32.shape[-1]
            cp("act", xb[:, 0:n], src_f32, [src_res], [res("xb")])
            b = gbank()
            mm(ps[:, b, 0:n], rm[:, typ, :], xb[:, 0:n], True, True, [res("xb"), rc], [psr[b]])
            tt("pool", t1[:, 0:n], src_f32, cos, ALU.mult, [src_res, cs_res], [res("t1")])
            tt("dve", t2[:, 0:n], ps[:, b, 0:n], sin, ALU.mult, [psr[b], cs_res], [res("t2")])
            tt("pool", out_bf, t1[0:nrows, 0:n], t2[0:nrows, 0:n], ALU.add, [res("t1"), res("t2")], [out_res])

        def _chunk_vars(c):
            return hTc[c % 2], r_hTc[c % 2], c * 512, cst[c % 2], res("cst%d" % (c % 2))

        def xproc_gen(c):
            hc, rh, t0, csb, r_cs = _chunk_vars(c)
            dma("sp", csb, tabs[1, :, :, t0:t0 + 512].rearrange("s p n -> p s n"), [], [r_cs], "cst%d" % (c % 2))
            for q4 in range(4):
                ti = c * 4 + q4
                xx = xt[ti % 2]
                rx = r_xt[ti % 2]
                dma("sp", xx, x[ti * 128:(ti + 1) * 128, :], [], [rx], "xt%d" % (ti % 2))
                act(xn, xx, AF.Square, [rx], [res("xn"), res("st1")], accum=st1[:, 0:1])
                act(st1[:, 1:2], st1[:, 0:1], AF.Sqrt, [res("st1")], [res("st1b")], scale=1.0 / D, bias=EPS)
                recip(st1[:, 2:3], st1[:, 1:2], [res("st1b")], [res("st1c")])
                ts("dve", xn, xx, st1[:, 2:3], None, ALU.mult, None, [rx, res("st1c")], [res("xn")])
                for half in range(2):
                    b = gbank()
                    pb = psb(b)
                    for k8 in range(8):
                        kc = half * 8 + k8
                        tr(pb[:, k8 * 128:(k8 + 1) * 128], xn[:, kc * 128:(kc + 1) * 128], [res("xn"), rc], [psr[b]])
                    for k8 in range(8):
                        kc = half * 8 + k8
                        eng = "dve" if k8 % 2 == 0 else "pool"
                        if eng == "pool":
                            act(hc[:, kc, q4 * 128:(q4 + 1) * 128], pb[:, k8 * 128:(k8 + 1) * 128], AF.Identity, [psr[b], res("AB")], [rh], scale=A1[:, kc:kc + 1], bias=B1[:, kc:kc + 1])
                        else:
                            ts("dve", hc[:, kc, q4 * 128:(q4 + 1) * 128], pb[:, k8 * 128:(k8 + 1) * 128], A1[:, kc:kc + 1], B1[:, kc:kc + 1], ALU.mult, ALU.add, [psr[b], res("AB")], [rh])
                yield
            dma("act", hT_scr[:, :, t0:t0 + 512], hc, [rh], [res("hT_scr")], "hTs%d" % (c % 2))

            yield

        def proj_gen(c):
            hc, rh, t0, csb, r_cs = _chunk_vars(c)
            def proj(c0, M, b):
                for kc in range(16):
                    mm(ps[0:M, b, :], Wa[:, kc, c0:c0 + M], hc[:, kc, :], kc == 0, kc == 15, [r_Wa, rh], [psr[b]])

            sb_ = 5
            for j in range(4 if c >= OC0 else 0):
                b = gbank()
                proj(CQ0 + j * 128, 128, b)
                act(sq, ps[:, b, :], AF.Square, [psr[b]], [res("sq")])
                mm(ps[:, sb_, :], ones[:], sq, j == 0, j == 3, [res("sq"), rc], [psr[sb_]])
                act(cqg[:, j, :], ps[:, b, :], AF.Identity, [psr[b], rc], [res("cqg")], scale=gq[:, j:j + 1])
                yield
            if c >= OC0:
                act(rstdb, ps[:, sb_, :], AF.Sqrt, [psr[sb_]], [res("rstdb")], scale=1.0 / 512, bias=EPS)
                recip(rstdb, rstdb, [res("rstdb")], [res("rstdb")])
                dma("pool", rstdq_scr[0:1, t0:t0 + 512], rstdb[0:1, :], [res("rstdb")], [res("rstdq_scr")], "rq")
                dma("act", cqg_scr[:, :, t0:t0 + 512], cqg, [res("cqg")], [res("cqg_scr")], "cqgs")
            bk = [6, 7]
            for j in range(2):
                proj(CKV0 + j * 128, 128, bk[j])
                act(sq, ps[:, bk[j], :], AF.Square, [psr[bk[j]]], [res("sq")])
                mm(ps[:, sb_, :], ones[:], sq, j == 0, j == 1, [res("sq"), rc], [psr[sb_]])
                yield
            act(rstdb, ps[:, sb_, :], AF.Sqrt, [psr[sb_]], [res("rstdb")], scale=1.0 / 256, bias=EPS)
            recip(rstdb, rstdb, [res("rstdb")], [res("rstdb")])
            for j in range(2):
                stt(ckvn[:, j, :], ps[:, bk[j], :], gkv[:, j:j + 1], rstdb, ALU.mult, ALU.mult, [psr[bk[j]], res("rstdb"), rc], [res("ckvn")])
            dma("pool", ckvT_scr[:, :, t0:t0 + 512], ckvn, [res("ckvn")], [res("ckvT_scr")], "ckvs")
            b = gbank()
            proj(KRI0, 112, b)
            cp("act", xq[0:112, :], ps[0:112, b, :], [psr[b]], [res("xq")])
            dma("act", wiT_scr[:, t0:t0 + 512], xq[96:112, :], [res("xq")], [res("wiT_scr")], "wis")
            cp("act", xb[0:112, :], xq[0:112, :], [res("xq")], [res("xb")])
            b2 = gbank()
            mm(ps[0:112, b2, :], rm[0:112, 1, 0:112], xb[0:112, :], True, True, [res("xb"), rc], [psr[b2]])
            tt("pool", t1[0:112, :], xq[0:112, :], csb[0:112, 0, :], ALU.mult, [res("xq"), r_cs], [res("t1")])
            tt("dve", t2[0:112, :], ps[0:112, b2, :], csb[0:112, 1, :], ALU.mult, [psr[b2], r_cs], [res("t2")])
            tt("pool", krio[0:96, :], t1[0:96, :], t2[0:96, :], ALU.add, [res("t1"), res("t2")], [res("krio")])
            dma("pool", kri_scr[:, t0:t0 + 512], krio[0:96, :], [res("krio")], [res("kri_scr")], "kris")
            yield
            for h in range(8):
                b = gbank()
                mm(ps[:, b, :], WukT[:, 0, h, :], ckvn[:, 0, :], True, False, [r_W2, res("ckvn")], [psr[b]])
                mm(ps[:, b, :], WukT[:, 1, h, :], ckvn[:, 1, :], False, False, [r_W2, res("ckvn")], [psr[b]])
                mm(ps[:, b, :], Esel[0:96, :], krio[0:96, :], False, True, [r_W2, res("krio")], [psr[b]])
                cp("act" if h % 2 else "dve", KhTc[:, h, :], ps[:, b, :], [psr[b]], [res("KhTc")])
                yield
            dma("act", KhT_scr[:, :, t0:t0 + 512], KhTc, [res("KhTc")], [res("KhT_scr")], "khs")
            for q4 in range(4):
                for n2 in range(2):
                    b = gbank()
                    for j in range(2):
                        mm(ps[:, b, :], ckvn[:, j, q4 * 128:(q4 + 1) * 128], Wuv2[:, j].rearrange("p h v -> p (h v)")[:, n2 * 512:(n2 + 1) * 512], j == 0, j == 1, [res("ckvn"), r_W2], [psr[b]])
                    cp("act" if n2 else "dve", VHc[:, q4, n2 * 512:(n2 + 1) * 512], ps[:, b, :], [psr[b]], [res("VHc")])
                    yield
            for h in range(8):
                dma("act", VH_scr[h, :, 4 * c:4 * c + 4, :], VHc[:, :, h * 128:(h + 1) * 128], [res("VHc")], [res("VH_scr")], "vhs")
            for j in range(8 if c >= OC0 else 0):
                b = gbank()
                proj(GA0 + j * 128, 128, b)
                g = gao[j % 2]
                act(g, ps[:, b, :], AF.Silu, [psr[b]], [res("gao%d" % (j % 2))])
                dma("act", gaT_scr[:, j, t0:t0 + 512], g, [res("gao%d" % (j % 2))], [res("gaT_scr")], "gas%d" % (j % 2))
                yield

            yield

        def _drain(gen):
            for _ in gen:
                pass

        _drain(xproc_gen(0))
        for c in range(NCH):
            pg = proj_gen(c)
            xg = xproc_gen(c + 1) if c + 1 < NCH else None
            n_ = 0
            for _ in pg:
                n_ += 1
                if xg is not None and n_ % 3 == 0:
                    next(xg, None)
            if xg is not None:
                _drain(xg)
        S.barrier()

        AR.reset()
        kiT = AR.alloc([128, T], BF16)
        Wqi = AR.alloc([128, 4, 1024], BF16)
        cq2 = [AR.alloc([128, 4, 128], BF16) for _ in range(2)]
        cs2 = [AR.alloc([128, 2, 4, 128], F32) for _ in range(2)]
        wtok = [AR.alloc([128, 16], F32) for _ in range(2)]
        rcol = [AR.alloc([128, 2], F32) for _ in range(2)]
        wsc = [AR.alloc([128, 16], F32) for _ in range(2)]
        xq3 = [AR.alloc([128, 512], F32) for _ in range(2)]
        xb3 = [AR.alloc([128, 512], BF16) for _ in range(2)]
        t13 = [AR.alloc([128, 512], F32) for _ in range(2)]
        t23 = [AR.alloc([128, 512], F32) for _ in range(2)]
        qiT = [AR.alloc([128, 8, 128], BF16) for _ in range(2)]
        Dm = [AR.alloc([128, 16, 128], BF16) for _ in range(2)]
        NRL = 4
        rl = [AR.alloc([128, 2, 512], BF16) for _ in range(NRL)]
        isc = [AR.alloc([128, T], F32) for _ in range(2)]
        junk = AR.alloc([128, T], BF16)
        m01 = [AR.alloc([128, T], BF16) for _ in range(2)]
        mT = [AR.alloc([128, NTT, 128], BF16) for _ in range(2)]
        bs = [AR.alloc([128, 8], F32) for _ in range(2)]
        r_kiT, r_Wqi = res("kiT"), res("Wqi")
        dma("sp", kiT[0:64, :], kri_scr[32:96, :], [res("kri_scr")], [r_kiT], "p3Ksp")
        dma("sp", kiT[64:128, :], kri_scr[32:96, :], [res("kri_scr")], [r_kiT], "p3Ksp")
        dma("pool", Wqi, w_uqi.rearrange("(f p) n -> p f n", p=128), [], [r_Wqi], "p3Kpool")
        ISBS = [6, 7]
        isb_cnt = [0]
        rl_cnt = [0]
        pair_cnt = [0]
        LA = 3

        def p3_loads(qb):
            t0 = qb * 128
            s = qb % 2
            r_in = res("p3in%d" % s)
            dma("sp", cq2[s], cqg_scr[:, :, t0:t0 + 128], [res("cqg_scr")], [r_in], "p3in%d" % s)
            for s_ in range(2):
                dma("sp", cs2[s][:, s_, :, :], tabs[2, s_, :, t0:t0 + 128].unsqueeze(1).to_broadcast([128, 4, 128]), [], [r_in], "p3in%d" % s)
            dma("sp", wtok[s], wiT_scr[:, t0:t0 + 128].rearrange("h p -> p h"), [res("wiT_scr")], [r_in], "p3in%d" % s, slow=True)
            dma("sp", rcol[s][:, 0:1], rstdq_scr[0:1, t0:t0 + 128].rearrange("o p -> p o"), [res("rstdq_scr")], [r_in], "p3in%d" % s, slow=True)

        def p3_prologue(qb):
            s = qb % 2
            r_in = res("p3in%d" % s)
            r_q = res("qiT%d" % s)
            for half in range(2):
                hs = half
                b = gbank()
                for jj in range(4):
                    j = half * 4 + jj
                    for f in range(4):
                        mm(ps[:, b, jj * 128:(jj + 1) * 128], Wqi[:, f, j * 128:(j + 1) * 128], cq2[s][:, f, :], f == 0, f == 3, [r_Wqi, r_in], [psr[b]])
                cp("act", xb3[hs], ps[:, b, :], [psr[b]], [res("xb3%d" % hs)])
                cp("act", xq3[hs], ps[:, b, :], [psr[b]], [res("xq3%d" % hs)])
                b2 = gbank()
                mm(ps[:, b2, :], rm[:, 2, :], xb3[hs], True, True, [res("xb3%d" % hs), rc], [psr[b2]])
                tt("pool", t13[hs], xq3[hs], cs2[s][:, 0].rearrange("p a b -> p (a b)"), ALU.mult, [res("xq3%d" % hs), r_in], [res("t13%d" % hs)])
                tt("dve", t23[hs], ps[:, b2, :], cs2[s][:, 1].rearrange("p a b -> p (a b)"), ALU.mult, [psr[b2], r_in], [res("t23%d" % hs)])
                tt("pool", qiT[s][:, half * 4:half * 4 + 4, :].rearrange("p a b -> p (a b)"), t13[hs], t23[hs], ALU.add, [res("t13%d" % hs), res("t23%d" % hs)], [r_q])
            ts("pool", wsc[s], wtok[s], rcol[s][:, 0:1], 1.0 / 32, ALU.mult, ALU.mult, [r_in], [res("wsc%d" % s)])
            for h in range(16):
                ts("pool", Dm[s][:, h, :], ident[:], wsc[s][:, h:h + 1], 1.0, ALU.mult, ALU.mult, [rc, res("wsc%d" % s)], [res("Dm%d" % s)])

        def p3_groups(qb):
            s = qb % 2
            ng = qb // 4 + 1
            items = [(g, hp) for g in range(ng) for hp in range(8)]
            st = {}

            def stageA(g, hp):
                nt = min(4, qb + 1 - 4 * g)
                W = nt * 128
                k0 = g * 512
                pr = pair_cnt[0] % 3
                pair_cnt[0] += 1
                for e in range(2):
                    b = 2 * pr + e
                    mm(ps[:, b, 0:W], qiT[s][64 * e:64 * e + 64, hp, :], kiT[64 * e:64 * e + 64, k0:k0 + W], True, True, [res("qiT%d" % s), r_kiT], [psr[b]])
                ri = rl_cnt[0] % NRL
                rl_cnt[0] += 1
                act(rl[ri][:, :, 0:W], ps[:, 2 * pr:2 * pr + 2, 0:W], AF.Relu, [psr[2 * pr], psr[2 * pr + 1]], [res("rl%d" % ri)])
                st[(g, hp)] = ri

            def stageB(g, hp):
                nt = min(4, qb + 1 - 4 * g)
                W = nt * 128
                k0 = g * 512
                if hp == 0:
                    isb_cnt[0] += 1
                ISB = ISBS[isb_cnt[0] % 2]
                ri = st[(g, hp)]
                for e in range(2):
                    h = 2 * hp + e
                    mm(ps[:, ISB, 0:W], Dm[s][:, h, :], rl[ri][:, e, 0:W], h == 0, h == 15, [res("Dm%d" % s), res("rl%d" % ri)], [psr[ISB]])
                if hp == 7:
                    r_i = res("isc%d" % s)
                    if g == ng - 1:
                        cp("act", isc[s][:, k0:k0 + W], ps[:, ISB, 0:W], [psr[ISB]], [r_i])
                        tt("pool", isc[s][:, k0 + W - 128:k0 + W], isc[s][:, k0 + W - 128:k0 + W], cm[:], ALU.add, [r_i, rc], [r_i])
                    elif g < OC0:
                        act(isc[s][:, k0:k0 + W], ps[:, ISB, 0:W], AF.Identity, [psr[ISB], rc], [r_i], bias=padb[:, 0:1])
                    else:
                        cp("act", isc[s][:, k0:k0 + W], ps[:, ISB, 0:W], [psr[ISB]], [r_i])
            n = len(items)
            for i in range(n + LA):
                if i < n:
                    stageA(*items[i])
                if i - LA >= 0:
                    stageB(*items[i - LA])

        NIT3 = 15

        def p3_bisect(qb):
            s = qb % 2
            Sk = (qb + 1) * 128
            b_ = bs[s]
            r_i = res("isc%d" % s)
            r_bs = res("bs_%d" % s)
            memset("dve", b_[:, 0:1], 0.0, [r_bs])
            for it in range(NIT3):
                Wk = 16.0 / (2 ** it)
                ts("dve", junk[:, 0:Sk], isc[s][:, 0:Sk], b_[:, 0:1], None, ALU.is_ge, ALU.add, [r_i, r_bs], [res("junk"), res("bs2_%d" % s)], accum=b_[:, 2:3])
                ts("dve", b_[:, 3:4], b_[:, 2:3], 255.5, Wk, ALU.is_ge, ALU.mult, [res("bs2_%d" % s)], [res("bs3_%d" % s)])
                ts("dve", b_[:, 0:1], b_[:, 3:4], b_[:, 0:1], -Wk / 2, ALU.add, ALU.add, [r_bs, res("bs3_%d" % s)], [r_bs])
            ts("dve", b_[:, 1:2], b_[:, 0:1], -16.0 / (2 ** NIT3), None, ALU.add, None, [r_bs], [res("bs1_%d" % s)])
            ts("dve", m01[s][:, 0:Sk], isc[s][:, 0:Sk], b_[:, 1:2], None, ALU.is_ge, None, [r_i, res("bs1_%d" % s)], [res("m01_%d" % s)])

        def p3_transposes(qb):
            s = qb % 2
            mt = mT[s]
            r_mt = res("mT%d" % s)
            for g8 in range((qb + 8) // 8):
                n8 = min(8, qb + 1 - 8 * g8)
                b = gbank()
                pb = psb(b)
                for i in range(n8):
                    kt = g8 * 8 + i
                    tr(pb[:, i * 128:(i + 1) * 128], m01[s][:, kt * 128:(kt + 1) * 128], [res("m01_%d" % s), rc], [psr[b]])
                cp("act", mt[:, g8 * 8:g8 * 8 + n8, :].rearrange("p a b -> p (a b)"), pb[:, 0:n8 * 128], [psr[b]], [r_mt])
            c_, j_ = qb // 4, qb % 4
            nk_ = 4 * c_ + 4
            dma("act", maskT_scr[c_, :, 0:nk_, j_ * 128:(j_ + 1) * 128], mt[:, 0:nk_, :], [r_mt], [res("maskT_scr")], "mts%d" % s)

        memset("pool", mT[0][:], 0.0, [res("mT0")])
        memset("pool", mT[1][:], 0.0, [res("mT1")])
        p3_loads(OT0)
        p3_loads(OT0 + 1)
        p3_prologue(OT0)
        for qb in range(OT0, NTT):
            p3_groups(qb)
            if qb + 2 < NTT:
                p3_loads(qb + 2)
            if qb + 1 < NTT:
                p3_prologue(qb + 1)
            p3_bisect(qb)
            if qb >= OT0 + 1:
                p3_transposes(qb - 1)
        p3_transposes(NTT - 1)
        S.barrier()

        AR.reset()
        Wq = AR.alloc([128, 4, 1024], BF16)
        cq4 = AR.alloc([128, 4, 512], BF16)
        rs4 = AR.alloc([128, 512], F32)
        cs4 = AR.alloc([128, 2, 512], F32)
        mTc = AR.alloc([128, NTT, 512], BF16)
        ga4 = AR.alloc([128, 8, 512], BF16)
        xq4 = [AR.alloc([128, 512], F32) for _ in range(2)]
        xb4 = [AR.alloc([128, 512], BF16) for _ in range(2)]
        t14 = [AR.alloc([128, 512], F32) for _ in range(2)]
        t24 = [AR.alloc([128, 512], F32) for _ in range(2)]
        qh = AR.alloc([128, 8, 512], BF16)
        Kh = [AR.alloc([128, T], BF16) for _ in range(2)]
        VH = [AR.alloc([128, NTT, 128], BF16) for _ in range(2)]
        NPB = 4
        PT = [AR.alloc([128, 2, 512], BF16) for _ in range(NPB)]
        PmT = [AR.alloc([128, 2, 512], BF16) for _ in range(NPB)]
        rzb = [AR.alloc([128, 512], F32) for _ in range(2)]
        tmp4 = [AR.alloc([128, 512], F32) for _ in range(2)]
        oA = AR.alloc([128, 8, 512], BF16)
        r_K = res("Kres")
        dma("pool", Wq, w_uq.rearrange("(f p) n -> p f n", p=128), [], [r_K], "p4Kpool")
        ACCS = [[4, 5], [6, 7]]
        gb4 = [0]

        def gbank4():
            gb4[0] += 1
            return gb4[0] % 4
        pcnt = [0]
        hc4 = [0]
        LA4 = 3

        def p4_kv_load(c, h, slot):
            nkt = 4 * c + 4
            r_kv = res("KV%d" % slot)
            dma("sp", Kh[slot][:, 0:nkt * 128], KhT_scr[:, h, 0:nkt * 128], [res("KhT_scr")], [r_kv], "kv%d" % slot)
            dma("sp", VH[slot][:, 0:nkt, :], VH_scr[h, :, 0:nkt, :], [res("VH_scr")], [r_kv], "kv%d" % slot)

        heads_seq = [(c, h) for c in range(OC0, NCH) for h in range(8)]
        p4_kv_load(heads_seq[0][0], heads_seq[0][1], 0)
        for c in range(OC0, NCH):
            t0 = c * 512
            nkt = 4 * c + 4
            r_in = res("p4in")
            r_m = res("p4mask")
            dma("sp", cq4, cqg_scr[:, :, t0:t0 + 512], [res("cqg_scr")], [r_in], "p4in")
            dma("sp", rs4, rstdq_scr[0:1, t0:t0 + 512].partition_broadcast(128), [res("rstdq_scr")], [r_in], "p4in")
            dma("sp", cs4, tabs[0, :, :, t0:t0 + 512].rearrange("s p n -> p s n"), [], [r_in], "p4in")
            dma("sp", ga4, gaT_scr[:, :, t0:t0 + 512], [res("gaT_scr")], [r_in], "p4in")
            dma("sp", mTc[:, 0:nkt, :], maskT_scr[c, :, 0:nkt, :], [res("maskT_scr")], [r_m], "p4mask")
            r_qh = res("qh")
            pend = []
            for h in range(8):
                hs = h % 2
                b = gbank4()
                for f in range(4):
                    mm(ps[:, b, :], Wq[:, f, h * 128:(h + 1) * 128], cq4[:, f, :], f == 0, f == 3, [r_K, r_in], [psr[b]])
                for p_ in pend:
                    p_()
                pend = []
                tt("dve", xq4[hs], ps[:, b, :], rs4, ALU.mult, [psr[b], r_in], [res("xq4%d" % hs)])
                cp("act", xb4[hs], xq4[hs], [res("xq4%d" % hs)], [res("xb4%d" % hs)])

                def part2(h=h, hs=hs):
                    b2 = gbank4()
                    mm(ps[:, b2, :], rm[:, 0, :], xb4[hs], True, True, [res("xb4%d" % hs), rc], [psr[b2]])
                    tt("pool", t14[hs], xq4[hs], cs4[:, 0, :], ALU.mult, [res("xq4%d" % hs), r_in], [res("t14%d" % hs)])
                    tt("dve", t24[hs], ps[:, b2, :], cs4[:, 1, :], ALU.mult, [psr[b2], r_in], [res("t24%d" % hs)])
                    tt("pool", qh[:, h, :], t14[hs], t24[hs], ALU.add, [res("t14%d" % hs), res("t24%d" % hs)], [r_qh])
                pend.append(part2)
            for p_ in pend:
                p_()
            items = [(h, kp) for h in range(8) for kp in range(nkt // 2)]
            st4 = {}
            slot_of = {}

            def stageA(h, kp):
                if kp == 0:
                    slot_of[h] = hc4[0] % 2
                    hc4[0] += 1
                if kp == LA4:
                    idx = heads_seq.index((c, h))
                    if idx + 1 < len(heads_seq):
                        p4_kv_load(heads_seq[idx + 1][0], heads_seq[idx + 1][1], hc4[0] % 2)
                sl = slot_of[h]
                pr = 2 * (pcnt[0] % 2)
                for e in range(2):
                    kt = 2 * kp + e
                    mm(ps[:, pr + e, :], Kh[sl][:, kt * 128:(kt + 1) * 128], qh[:, h, :], True, True, [res("KV%d" % sl), r_qh], [psr[pr + e]])
                pi = pcnt[0] % NPB
                pcnt[0] += 1
                act(PT[pi], ps[:, pr:pr + 2, :], AF.Exp, [psr[pr], psr[pr + 1]], [res("PT%d" % pi)], scale=SCALE)
                tt("pool" if (kp < 2 or pcnt[0] % 4 == 0) else "dve", PmT[pi], PT[pi], mTc[:, 2 * kp:2 * kp + 2, :], ALU.mult, [res("PT%d" % pi), r_m], [res("PmT%d" % pi)])
                st4[(h, kp)] = pi

            def stageB(h, kp):
                pi = st4[(h, kp)]
                sl = slot_of[h]
                ACC = ACCS[sl]
                for e in range(2):
                    kt = 2 * kp + e
                    mm(ps[:, ACC[0], :], VH[sl][:, kt, :], PmT[pi][:, e, :], kt == 0, kt == nkt - 1, [res("KV%d" % sl), res("PmT%d" % pi)], [psr[ACC[0]]])
                for e in range(2):
                    kt = 2 * kp + e
                    mm(ps[:, ACC[1], :], ones[:], PmT[pi][:, e, :], kt == 0, kt == nkt - 1, [rc, res("PmT%d" % pi)], [psr[ACC[1]]])
                if kp == nkt // 2 - 1:
                    recip(rzb[sl], ps[:, ACC[1], :], [psr[ACC[1]]], [res("rzb%d" % sl)])
                    tt("dve", tmp4[sl], ps[:, ACC[0], :], rzb[sl], ALU.mult, [psr[ACC[0]], res("rzb%d" % sl)], [res("tmp4%d" % sl)])
                    tt("pool", oA[:, h, :], tmp4[sl], ga4[:, h, :], ALU.mult, [res("tmp4%d" % sl), r_in], [res("oA")])
            n = len(items)
            for i in range(n + LA4):
                if i < n:
                    stageA(*items[i])
                if i - LA4 >= 0:
                    stageB(*items[i - LA4])
            dma("pool", oT_scr[:, 0:8, t0:t0 + 512], oA, [res("oA")], [res("oT_scr")], "oas")
        S.barrier()

        AR.reset()
        HT = T // 2
        Wb = [AR.alloc([128, 16, 4, 128], BF16) for _ in range(2)]
        hT5 = [AR.alloc([128, 16, 512], BF16) for _ in range(2)]
        cs5 = [AR.alloc([128, 2, 512], F32) for _ in range(2)]
        qT = [AR.alloc([128, HT], BF16) for _ in range(2)]
        kT = [AR.alloc([128, T], BF16) for _ in range(2)]
        vT = [AR.alloc([128, T], BF16) for _ in range(2)]
        gT = [AR.alloc([128, HT], BF16) for _ in range(2)]
        VB = AR.alloc([128, 32, 128], BF16)
        xq5 = [AR.alloc([128, 512], F32) for _ in range(2)]
        xb5 = [AR.alloc([128, 512], BF16) for _ in range(2)]
        t15 = [AR.alloc([128, 512], F32) for _ in range(2)]
        t25 = [AR.alloc([128, 512], F32) for _ in range(2)]
        NP5 = 4
        PT5 = [AR.alloc([128, 512], BF16) for _ in range(NP5)]
        Pm5 = [AR.alloc([128, 512], BF16) for _ in range(NP5)]
        accO = AR.alloc([128, HT], F32)
        accZ = AR.alloc([128, HT], F32)
        oB = AR.alloc([128, HT], BF16)
        COLS = [1904, 2928, 3952, 4976]
        rcnt = [0]
        p5cnt = [0]
        LA5 = 3

        def load_wb(h):
            for i in range(4):
                dma("pool", Wb[h % 2][:, :, i, :], w_in[:, COLS[i] + 128 * h:COLS[i] + 128 * h + 128].rearrange("(kc p) n -> p kc n", p=128), [], [res("Wb%d" % (h % 2))], "wb%d" % (h % 2))

        def proj_gen(h):
            s5 = h % 2
            wbh = Wb[s5]
            r_wb = res("Wb%d" % s5)
            if h + 1 < 8:
                load_wb(h + 1)
            pend5 = []
            for c in range(NCH):
                t0 = c * 512
                hc = hT5[c % 2]
                rh = res("hT5_%d" % (c % 2))
                dma("sp", hc, hT_scr[:, :, t0:t0 + 512], [res("hT_scr")], [rh], "h5_%d" % (c % 2))
                csb = cs5[c % 2]
                r_cs = res("cs5_%d" % (c % 2))
                dma("sp", csb, tabs[0, :, :, t0:t0 + 512].rearrange("s p n -> p s n"), [], [r_cs], "c5_%d" % (c % 2))
                for i in range(4):
                    if c < OC0 and i in (0, 3):
                        continue
                    b = gbank()
                    for kc in range(16):
                        mm(ps[:, b, :], wbh[:, kc, i, :], hc[:, kc, :], kc == 0, kc == 15, [r_wb, rh], [psr[b]])
                    for p_ in pend5:
                        p_()
                    pend5 = []
                    if i < 2:
                        if i == 0:
                            dst = qT[s5][:, t0 - HT:t0 - HT + 512]
                            r_dst = res("qT%d" % s5)
                        else:
                            dst = kT[s5][:, t0:t0 + 512]
                            r_dst = res("kT%d" % s5)
                        ri = rcnt[0] % 2
                        rcnt[0] += 1
                        cp("act", xb5[ri], ps[:, b, :], [psr[b]], [res("xb5%d" % ri)])
                        cp("act", xq5[ri], ps[:, b, :], [psr[b]], [res("xq5%d" % ri)])

                        def part2(ri=ri, dst=dst, csb=csb, r_cs=r_cs, r_dst=r_dst):
                            b2 = gbank()
                            mm(ps[:, b2, :], rm[:, 0, :], xb5[ri], True, True, [res("xb5%d" % ri), rc], [psr[b2]])
                            tt("pool", t15[ri], xq5[ri], csb[:, 0, :], ALU.mult, [res("xq5%d" % ri), r_cs], [res("t15%d" % ri)])
                            tt("dve", t25[ri], ps[:, b2, :], csb[:, 1, :], ALU.mult, [psr[b2], r_cs], [res("t25%d" % ri)])
                            tt("pool", dst, t15[ri], t25[ri], ALU.add, [res("t15%d" % ri), res("t25%d" % ri)], [r_dst])
                        pend5.append(part2)
                    elif i == 2:
                        cp("dve", vT[s5][:, t0:t0 + 512], ps[:, b, :], [psr[b]], [res("vT%d" % s5)])
                    else:
                        act(gT[s5][:, t0 - HT:t0 - HT + 512], ps[:, b, :], AF.Silu, [psr[b]], [res("gT%d" % s5)])
                    yield
            for p_ in pend5:
                p_()
            yield

        def attn_gen(h):
            s5 = h % 2
            r_q, r_k, r_v, r_g = res("qT%d" % s5), res("kT%d" % s5), res("vT%d" % s5), res("gT%d" % s5)
            first = True
            for d in (1, 4, 16):
                nblk = T // (128 * d)
                for g8 in range(4):
                    b = gbank()
                    pb = psb(b)
                    for i in range(8):
                        ti = g8 * 8 + i
                        r_, bl = ti // nblk, ti % nblk
                        st_ = r_ + d * 128 * bl
                        src = vT[s5][:, st_:st_ + 127 * d + 1:d]
                        tr(pb[:, i * 128:(i + 1) * 128], src, [r_v, rc], [psr[b]])
                    cp("act" if g8 % 2 else "dve", VB[:, g8 * 8:g8 * 8 + 8, :].rearrange("p a b -> p (a b)"), pb[:, 0:1024], [psr[b]], [res("VB")])
                    yield
                qtiles = [(r_, bl) for r_ in range(d) for bl in range(nblk // 2, nblk)]
                pairs = [qtiles[i:i + 2] for i in range(0, 16, 2)]
                stp = {}

                def stageA(pi_, pair):
                    b = gbank()
                    slots = []
                    for qi_, (r_, bl) in enumerate(pair):
                        st_q = r_ + d * 128 * bl - HT
                        qap = qT[s5][:, st_q:st_q + 127 * d + 1:d]
                        for kk in range(2):
                            kb_ = bl - 1 + kk
                            sl = qi_ * 2 + kk
                            st_k = r_ + d * 128 * kb_
                            kap = kT[s5][:, st_k:st_k + 127 * d + 1:d]
                            mm(ps[:, b, sl * 128:(sl + 1) * 128], kap, qap, True, True, [r_k, r_q], [psr[b]])
                            slots.append((sl, qi_, r_ * nblk + kb_))
                    pp = p5cnt[0] % NP5
                    p5cnt[0] += 1
                    act(PT5[pp], ps[:, b, :], AF.Exp, [psr[b]], [res("PT5%d" % pp)], scale=SCALE)
                    npad = sum(1 for (r_, bl) in pair if bl == nblk // 2)
                    bsel = 0 if npad == 0 else (2 if npad == 2 else 1)
                    assert npad != 1 or pair[0][1] == nblk // 2
                    tt("pool" if pp % 2 else "dve", Pm5[pp], PT5[pp], band[:, bsel, :], ALU.mult, [res("PT5%d" % pp), rc], [res("Pm5%d" % pp)])
                    stp[pi_] = (pp, slots)

                def stageB(pi_, pair, first=first):
                    pp, slots = stp[pi_]
                    b3 = gbank()
                    for qi_, (r_, bl) in enumerate(pair):
                        mine = [s_ for s_ in slots if s_[1] == qi_]
                        for n_, (sl, _, vt_) in enumerate(mine):
                            mm(ps[:, b3, qi_ * 128:(qi_ + 1) * 128], VB[:, vt_, :], Pm5[pp][:, sl * 128:(sl + 1) * 128], n_ == 0, n_ == len(mine) - 1, [res("VB"), res("Pm5%d" % pp)], [psr[b3]])
                        for n_, (sl, _, vt_) in enumerate(mine):
                            mm(ps[:, b3, 256 + qi_ * 128:256 + (qi_ + 1) * 128], ones[:], Pm5[pp][:, sl * 128:(sl + 1) * 128], n_ == 0, n_ == len(mine) - 1, [rc, res("Pm5%d" % pp)], [psr[b3]])
                    for qi_, (r_, bl) in enumerate(pair):
                        st_q = r_ + d * 128 * bl - HT
                        sl_o = slice(st_q, st_q + 127 * d + 1, d)
                        if first:
                            cp("act", accO[:, sl_o], ps[:, b3, qi_ * 128:(qi_ + 1) * 128], [psr[b3]], [res("accO")])
                            cp("act", accZ[:, sl_o], ps[:, b3, 256 + qi_ * 128:256 + (qi_ + 1) * 128], [psr[b3]], [res("accZ")])
                        else:
                            tt("dve", accO[:, sl_o], accO[:, sl_o], ps[:, b3, qi_ * 128:(qi_ + 1) * 128], ALU.add, [psr[b3], res("accO")], [res("accO")])
                            tt("dve", accZ[:, sl_o], accZ[:, sl_o], ps[:, b3, 256 + qi_ * 128:256 + (qi_ + 1) * 128], ALU.add, [psr[b3], res("accZ")], [res("accZ")])
                n = len(pairs)
                for i in range(n + LA5):
                    if i < n:
                        stageA(i, pairs[i])
                    if i - LA5 >= 0:
                        stageB(i - LA5, pairs[i - LA5])
                    yield
                first = False
            for c4 in range(4):
                sl_ = slice(c4 * 512, (c4 + 1) * 512)
                recip(accZ[:, sl_], accZ[:, sl_], [res("accZ")], [res("accZ")])
                tt("dve", accO[:, sl_], accO[:, sl_], accZ[:, sl_], ALU.mult, [res("accO"), res("accZ")], [res("accO")])
                tt("pool", oB[:, sl_], accO[:, sl_], gT[s5][:, sl_], ALU.mult, [res("accO"), r_g], [res("oB")])
                yield
            dma("pool", oT_scr[:, 8 + h, HT:T], oB, [res("oB")], [res("oT_scr")], "obs")
            yield

        def drain(gen):
            for _ in gen:
                pass

        load_wb(0)
        drain(proj_gen(0))
        for h in range(8):
            ag = attn_gen(h)
            if h + 1 < 8:
                pg = proj_gen(h + 1)
                for _ in pg:
                    next(ag, None)
                    next(ag, None)
            drain(ag)
        S.barrier()

        AR.reset()
        Wo = AR.alloc([128, 16, D], BF16)
        oT6 = [AR.alloc([128, 16, 128], BF16) for _ in range(2)]
        x6 = [AR.alloc([128, D], F32) for _ in range(2)]
        y6 = [AR.alloc([128, D], F32) for _ in range(2)]
        jk6 = AR.alloc([128, 512], BF16)
        s6 = AR.alloc([128, 16], F32)
        r_Wo = res("Wo")
        for i in range(4):
            dma("pool", Wo[:, :, i * 512:(i + 1) * 512], w_out[:, i * 512:(i + 1) * 512].rearrange("(kc p) n -> p kc n", p=128), [], [r_Wo], "wo")
        fin = []
        for ti in range(OT0, NTT):
            t0 = ti * 128
            s = ti % 2
            YB = [4, 5, 6, 7] if s else [0, 1, 2, 3]
            r_in = res("p6in%d" % s)
            dma("sp", oT6[s], oT_scr[:, :, t0:t0 + 128], [res("oT_scr")], [r_in], "p6a%d" % s)
            dma("sp", x6[s], x[t0:t0 + 128, :], [], [res("x6_%d" % s)], "p6b%d" % s)
            for n in range(4):
                for kc in range(16):
                    mm(ps[:, YB[n], :], oT6[s][:, kc, :], Wo[:, kc, n * 512:(n + 1) * 512], kc == 0, kc == 15, [r_in, r_Wo], [psr[YB[n]]])
                act(jk6, ps[:, YB[n], :], AF.Square, [psr[YB[n]]], [res("jk6"), res("s6_%d" % n)], accum=s6[:, n:n + 1])
            tt("dve", s6[:, 4:5], s6[:, 0:1], s6[:, 1:2], ALU.add, [res("s6_0"), res("s6_1")], [res("s6a")])
            tt("dve", s6[:, 5:6], s6[:, 2:3], s6[:, 3:4], ALU.add, [res("s6_2"), res("s6_3")], [res("s6b")])
            tt("dve", s6[:, 6:7], s6[:, 4:5], s6[:, 5:6], ALU.add, [res("s6a"), res("s6b")], [res("s6c")])
            act(s6[:, 7:8], s6[:, 6:7], AF.Sqrt, [res("s6c")], [res("s6d")], scale=1.0 / D, bias=EPS)
            recip(s6[:, 8:9], s6[:, 7:8], [res("s6d")], [res("s6e")])
            yy = y6[s]
            r_yy = res("y6_%d" % s)
            for n in range(4):
                sl_ = slice(n * 512, (n + 1) * 512)
                stt(yy[:, sl_], ps[:, YB[n], :], s6[:, 8:9], Gbc[:, sl_], ALU.mult, ALU.mult, [psr[YB[n]], res("s6e"), res("Gbc")], [r_yy])
                tt("pool", yy[:, sl_], yy[:, sl_], x6[s][:, sl_], ALU.add, [r_yy, res("x6_%d" % s)], [r_yy])
            fin.append(dma("pool", out[t0 - OT0 * 128:t0 - OT0 * 128 + 128, :], yy, [r_yy], [res("out")], "outs%d" % s))
        S.emit(nc, final_waits=fin[-2:])
    return nc


def _host_inputs(inp):
    ident = np.eye(128, dtype=np.float32)
    f = lambda a: np.ascontiguousarray(np.asarray(a, dtype=np.float32))
    shared = {
        "w_ada": f(inp["w_ada"][0]),
        "bada_col": f(np.asarray(inp["b_ada"][0])[:4096].reshape(32, 128).T),
        "bada_g": f(np.asarray(inp["b_ada"][0])[4096:].reshape(1, D)),
        "gpre_col": f(np.asarray(inp["g_pre"][0]).reshape(16, 128).T),
        "gpost": f(np.asarray(inp["g_post"][0]).reshape(1, D)),
        "gq_col": f(np.asarray(inp["g_q"][0]).reshape(4, 128).T),
        "gkv_col": f(np.asarray(inp["g_kv"][0]).reshape(2, 128).T),
        "w_in": f(inp["w_in"][0]),
        "w_uq": f(inp["w_uq"][0]),
        "w_uqi": f(inp["w_uq_idx"][0]),
        "w_uk": f(inp["w_uk"][0]),
        "w_uv": f(inp["w_uv"][0]),
        "w_out": f(inp["w_out"][0]),
        "ident": ident,
    }
    per_g = []
    for g in range(2):
        tabs, rmats = _rope_tables(g)
        band, cm = _masks(g)
        padb = np.full((128, 512), 0.0 if g == 1 else NEG, np.float32)
        per_g.append({"tabs": tabs, "rmats": rmats, "band": f(band), "cm": f(cm), "padb": padb})
    maps = []
    xx = np.asarray(inp["x"], dtype=np.float32)
    cc = np.asarray(inp["c"], dtype=np.float32)
    for b in range(4):
        for g in range(2):
            m = dict(shared)
            m.update(per_g[g])
            if g == 1:
                m["x"] = np.ascontiguousarray(xx[b])
            else:
                xw = np.zeros((T, D), np.float32)
                xw[T // 2:] = xx[b, :T // 2]
                m["x"] = xw
            m["scol"] = np.ascontiguousarray(cc[b].reshape(16, 128).T)
            maps.append(m)
    return maps


_NC = None


def kernel(**inputs):
    global _NC
    if _NC is None:
        _NC = build()
    maps = _host_inputs(inputs)
    res_ = run_bass_kernel_spmd(_NC, maps, core_ids=list(range(8)))
    out = np.empty((4, T, D), np.float32)
    for b in range(4):
        for g in range(2):
            out[b, g * (T // 2):(g + 1) * (T // 2)] = np.asarray(res_.results[2 * b + g]["out"], dtype=np.float32)
    return out
```
